# Optimizing a Trainium2 kernel written in Bass

```python
import jax, jax.numpy as jnp
from jax import lax
import numpy as np

D_MODEL = 1024
BATCH = 4
SEQ = 8192
DEPTH = 1

HEAD_DIM = 64
SB_HEADS = 8
DIL_GROUPS = ((128, 1), (512, 4), (2048, 16))
DIL_HEADS_PER_GROUP = 4
N_DIL_GROUPS = len(DIL_GROUPS)
DIL_HEADS = DIL_HEADS_PER_GROUP * N_DIL_GROUPS
SB_WIDTH = SB_HEADS * HEAD_DIM
DIL_WIDTH = DIL_HEADS * HEAD_DIM
DIL_OUT_WIDTH = DIL_HEADS_PER_GROUP * HEAD_DIM
IN_COLS = 3 * SB_WIDTH + 3 * DIL_WIDTH + 2 * D_MODEL
Q_BLOCK = 128
ROPE_THETA = 10000.0
PEER_HEADS = 8
PEER_NKEYS = 128
PEER_EXPERTS = PEER_NKEYS * PEER_NKEYS
PEER_QDIM = 256
PEER_HALF = PEER_QDIM // 2
PEER_TOPK = 16
PEER_TOKEN_CHUNK = 128
N_MOD = 6
EPS = 1e-6

kernel_name = "hybrid_stickbreak_dilated_peer_adaln"


def rms_norm(x, g):
    xf = x.astype(jnp.float32)
    y = xf * lax.rsqrt(jnp.mean(xf * xf, axis=-1, keepdims=True) + EPS)
    return (y * g.astype(jnp.float32)).astype(x.dtype)


def modulate(h, shift, scale):
    return h * (1.0 + scale[:, None, :]) + shift[:, None, :]


def rope(x, pos):
    half = HEAD_DIM // 2
    inv = ROPE_THETA ** (-jnp.arange(half, dtype=jnp.float32) / half)
    ang = pos.astype(jnp.float32)[:, None] * inv[None, :]
    cos = jnp.cos(ang)[None, :, None, :]
    sin = jnp.sin(ang)[None, :, None, :]
    xf = x.astype(jnp.float32)
    x1, x2 = xf[..., :half], xf[..., half:]
    return jnp.concatenate([x1 * cos - x2 * sin, x1 * sin + x2 * cos], axis=-1).astype(x.dtype)


def stick_breaking_attention(q, k, v):
    S = q.shape[1]
    scale = HEAD_DIM ** -0.5
    outs = []
    for blk in range(S // Q_BLOCK):
        t0 = blk * Q_BLOCK
        kl = t0 + Q_BLOCK
        qb, kb, vb = q[:, t0:kl], k[:, :kl], v[:, :kl]
        z = jnp.einsum('bqhd,bkhd->bhqk', qb, kb).astype(jnp.float32) * scale
        t_idx = t0 + jnp.arange(Q_BLOCK)
        s_idx = jnp.arange(kl)
        mask = s_idx[None, :] < t_idx[:, None]
        log_1m_beta = jnp.where(mask, jax.nn.log_sigmoid(-z), 0.0)
        after = lax.cumsum(log_1m_beta, axis=3, reverse=True) - log_1m_beta
        a = jnp.where(mask, jnp.exp(jax.nn.log_sigmoid(z) + after), 0.0)
        outs.append(jnp.einsum('bhqk,bkhd->bqhd', a.astype(v.dtype), vb))
    return jnp.concatenate(outs, axis=1)


def dilated_attention(q, k, v):
    B, S = q.shape[:2]
    scale = HEAD_DIM ** -0.5
    n_blocks = S // Q_BLOCK

    def block(blk):
        t = blk * Q_BLOCK + jnp.arange(Q_BLOCK)
        qb = lax.dynamic_slice_in_dim(q, blk * Q_BLOCK, Q_BLOCK, axis=1)
        outs, lses = [], []
        for g, (w, r) in enumerate(DIL_GROUPS):
            n = w // r + 1
            idx = t[:, None] - r * jnp.arange(n)[None, :]
            valid = idx >= 0
            idx_c = jnp.maximum(idx, 0)
            kg = jnp.take(k[:, :, g], idx_c, axis=1)
            vg = jnp.take(v[:, :, g], idx_c, axis=1)
            z = jnp.einsum('bqhd,bqnhd->bhqn', qb[:, :, g], kg).astype(jnp.float32) * scale
            z = jnp.where(valid[None, None], z, -jnp.inf)
            lse = jax.nn.logsumexp(z, axis=-1)
            p = jnp.exp(z - lse[..., None])
            outs.append(jnp.einsum('bhqn,bqnhd->bqhd', p.astype(vg.dtype), vg).astype(jnp.float32))
            lses.append(lse)
        wts = jax.nn.softmax(jnp.stack(lses, axis=0), axis=0)
        wts = jnp.transpose(wts, (0, 1, 3, 2))[..., None]
        return jnp.sum(wts * jnp.stack(outs, axis=0), axis=0).astype(q.dtype)

    out = lax.map(block, jnp.arange(n_blocks))
    return jnp.transpose(out, (1, 0, 2, 3, 4)).reshape(B, S, DIL_HEADS_PER_GROUP, HEAD_DIM)


def peer_ffn(h, w_pq, peer_keys, peer_u, peer_v):
    B, S, D = h.shape
    T = B * S
    ht = h.reshape(T, D)
    q = (ht @ w_pq).reshape(T, PEER_HEADS, 2, PEER_HALF)
    sub = jnp.einsum('thpc,hpnc->thpn', q, peer_keys).astype(jnp.float32)
    s_top, i_top = lax.top_k(sub, PEER_TOPK)
    cand_s = (s_top[:, :, 0, :, None] + s_top[:, :, 1, None, :]).reshape(T, PEER_HEADS, PEER_TOPK * PEER_TOPK)
    cand_i = (i_top[:, :, 0, :, None] * PEER_NKEYS + i_top[:, :, 1, None, :]).reshape(T, PEER_HEADS, PEER_TOPK * PEER_TOPK)
    best_s, pos = lax.top_k(cand_s, PEER_TOPK)
    expert = jnp.take_along_axis(cand_i, pos, axis=-1)
    gate = jax.nn.softmax(best_s, axis=-1)
    hk = PEER_HEADS * PEER_TOPK
    n_chunks = T // PEER_TOKEN_CHUNK

    def chunk(args):
        xc, ec, gc = args
        u = jnp.take(peer_u, ec, axis=0)
        act = jax.nn.gelu(jnp.einsum('cd,ckd->ck', xc, u))
        vv = jnp.take(peer_v, ec, axis=0)
        return jnp.einsum('ck,ckd->cd', gc.astype(xc.dtype) * act, vv)

    out = lax.map(chunk, (ht.reshape(n_chunks, PEER_TOKEN_CHUNK, D),
                          expert.reshape(n_chunks, PEER_TOKEN_CHUNK, hk),
                          gate.reshape(n_chunks, PEER_TOKEN_CHUNK, hk)))
    return out.reshape(B, S, D)


def setup_inputs(seed: int = 0) -> dict:
    key = jax.random.key(seed)
    ks = jax.random.split(key, 16)
    D = D_MODEL
    f = jnp.float32
    nrm = lambda k, shape, s: jax.random.normal(k, shape, f) * s
    return {
        "x": nrm(ks[0], (BATCH, SEQ, D), 1.0),
        "c": nrm(ks[1], (BATCH, D), 1.0),
        "w_ada": nrm(ks[2], (DEPTH, D, N_MOD * D), 0.5 * D ** -0.5),
        "b_ada": nrm(ks[3], (DEPTH, N_MOD * D), 0.01),
        "g_mix": 1.0 + nrm(ks[4], (DEPTH, D), 0.02),
        "w_in": nrm(ks[5], (DEPTH, D, IN_COLS), D ** -0.5),
        "w_sb_o": nrm(ks[6], (DEPTH, SB_WIDTH, D), SB_WIDTH ** -0.5),
        "w_dil_o": nrm(ks[7], (DEPTH, DIL_OUT_WIDTH, D), DIL_OUT_WIDTH ** -0.5),
        "w_out": nrm(ks[8], (DEPTH, D, D), D ** -0.5),
        "g_ffn": 1.0 + nrm(ks[9], (DEPTH, D), 0.02),
        "w_pq": nrm(ks[10], (DEPTH, D, PEER_HEADS * PEER_QDIM), D ** -0.5),
        "peer_keys": nrm(ks[11], (DEPTH, PEER_HEADS, 2, PEER_NKEYS, PEER_HALF), PEER_HALF ** -0.5),
        "peer_u": nrm(ks[12], (DEPTH, PEER_EXPERTS, D), D ** -0.5),
        "peer_v": nrm(ks[13], (DEPTH, PEER_EXPERTS, D), PEER_HEADS ** -0.5),
        "g_final": 1.0 + nrm(ks[14], (D,), 0.02),
    }


def reference(x, c, w_ada, b_ada, g_mix, w_in, w_sb_o, w_dil_o, w_out, g_ffn, w_pq,
              peer_keys, peer_u, peer_v, g_final):
    B, S, D = x.shape
    pos = jnp.arange(S)
    splits = np.cumsum([SB_WIDTH] * 3 + [DIL_WIDTH] * 3 + [D_MODEL]).tolist()
    for layer in range(DEPTH):
        mod = jax.nn.silu(c) @ w_ada[layer] + b_ada[layer]
        sh1, sc1, gt1, sh2, sc2, gt2 = jnp.split(mod, N_MOD, axis=-1)

        h = modulate(rms_norm(x, g_mix[layer]), sh1, sc1)
        proj = h @ w_in[layer]
        q_sb, k_sb, v_sb, q_d, k_d, v_d, gate_sb, gate_d = jnp.split(proj, splits, axis=-1)

        hs = (B, S, SB_HEADS, HEAD_DIM)
        o_sb = stick_breaking_attention(q_sb.reshape(hs), k_sb.reshape(hs), v_sb.reshape(hs))

        hd = (B, S, DIL_HEADS, HEAD_DIM)
        gd = (B, S, N_DIL_GROUPS, DIL_HEADS_PER_GROUP, HEAD_DIM)
        q_dr = rope(q_d.reshape(hd), pos).reshape(gd)
        k_dr = rope(k_d.reshape(hd), pos).reshape(gd)
        o_d = dilated_attention(q_dr, k_dr, v_d.reshape(gd))

        br_sb = o_sb.reshape(B, S, SB_WIDTH) @ w_sb_o[layer]
        br_d = o_d.reshape(B, S, DIL_OUT_WIDTH) @ w_dil_o[layer]
        merged = jax.nn.sigmoid(gate_sb) * br_sb + jax.nn.sigmoid(gate_d) * br_d
        x = x + gt1[:, None, :] * (merged @ w_out[layer])

        h2 = modulate(rms_norm(x, g_ffn[layer]), sh2, sc2)
        x = x + gt2[:, None, :] * peer_ffn(h2, w_pq[layer], peer_keys[layer], peer_u[layer], peer_v[layer])
    return rms_norm(x, g_final)
```

```python
import contextlib
import numpy as np
import concourse.bass as bass
import concourse.mybir as mybir
from concourse.bass_utils import run_bass_kernel_spmd

F32 = mybir.dt.float32
BF16 = mybir.dt.bfloat16
AF = mybir.ActivationFunctionType
ALU = mybir.AluOpType
AX = mybir.AxisListType

D = 1024
SEQ = 8192
NB = 64
NOWN = 32
EPS = 1e-6
ENGS = ("pe", "act", "dve", "pool", "sp")
DIL = ((1, 2), (4, 5), (16, 17))
SAME_ENGINE_SYNC = True
DBG_SKIP = set()


class _Ins:
    __slots__ = ("eng", "fn", "waits", "signal", "sig_count", "is_dma", "dsem", "dcount")

    def __init__(self, eng, fn, is_dma=False):
        self.eng = eng
        self.fn = fn
        self.waits = []
        self.signal = False
        self.sig_count = 0
        self.is_dma = is_dma
        self.dsem = None
        self.dcount = 0


class Sched:
    def __init__(self, nc):
        self.nc = nc
        self.q = {e: [] for e in ENGS}
        self.last_w = {}
        self.readers = {}
        self.dma_counts = {}

    @staticmethod
    def _key(r):
        return r if isinstance(r, (tuple, str, int)) else id(r)

    def _dep(self, ins, other):
        if other is None or other is ins:
            return
        if other.is_dma:
            ins.waits.append(("D", other.dsem, other.dcount))
        elif other.eng != ins.eng or (SAME_ENGINE_SYNC and ins.eng != "pe"):
            other.signal = True
            ins.waits.append(("E", other.eng, other))

    def _track(self, ins, reads, writes):
        for r in reads:
            self._dep(ins, self.last_w.get(self._key(r)))
        for w in writes:
            k = self._key(w)
            self._dep(ins, self.last_w.get(k))
            rd = self.readers.get(k)
            if rd:
                for o in rd[0].values():
                    self._dep(ins, o)
                for o in rd[1]:
                    self._dep(ins, o)
        for r in reads:
            rd = self.readers.setdefault(self._key(r), ({}, []))
            if ins.is_dma:
                rd[1].append(ins)
            else:
                rd[0][ins.eng] = ins
        for w in writes:
            k = self._key(w)
            self.last_w[k] = ins
            self.readers[k] = ({}, [])

    def op(self, eng, fn, reads=(), writes=()):
        ins = _Ins(eng, fn)
        self._track(ins, reads, writes)
        self.q[eng].append(ins)
        return ins

    def dma(self, eng, fn, reads=(), writes=(), sem=None):
        ins = _Ins(eng, fn, is_dma=True)
        k = self._key(sem)
        self.dma_counts[k] = self.dma_counts.get(k, 0) + 16
        ins.dsem = k
        ins.dcount = self.dma_counts[k]
        self._track(ins, reads, writes)
        self.q[eng].append(ins)
        return ins

    def barrier(self):
        dsnap = dict(self.dma_counts)
        lastc = {}
        for f in ENGS:
            j = len(self.q[f]) - 1
            while j >= 0 and (self.q[f][j].is_dma or self.q[f][j].fn is None):
                j -= 1
            lastc[f] = self.q[f][j] if j >= 0 else None
        for e in ENGS:
            ins = _Ins(e, None)
            for f in ENGS:
                if f != e and lastc[f] is not None:
                    lastc[f].signal = True
                    ins.waits.append(("E", f, lastc[f]))
            for k, c in dsnap.items():
                ins.waits.append(("D", k, c))
            self.q[e].append(ins)
        self.last_w = {}
        self.readers = {}

    def emit(self):
        nc = self.nc
        for e in ENGS:
            c = 0
            for ins in self.q[e]:
                if ins.signal:
                    c += 1
                    ins.sig_count = c
        dkeys = list(self.dma_counts.keys())
        with contextlib.ExitStack() as es:
            esem = {e: es.enter_context(nc.semaphore("s_" + e)) for e in ENGS}
            dsem = {k: es.enter_context(nc.semaphore("d%d" % i)) for i, k in enumerate(dkeys)}
            block = es.enter_context(nc.Block())
            final_d = dict(self.dma_counts)

            def make(e):
                def body(engobj):
                    known_e = {f: 0 for f in ENGS}
                    known_d = {}
                    for ins in self.q[e]:
                        for w in ins.waits:
                            if w[0] == "E":
                                f, c = w[1], w[2].sig_count
                                if known_e[f] >= c:
                                    continue
                                known_e[f] = c
                                engobj.wait_ge(esem[f], c)
                            else:
                                k, c = w[1], w[2]
                                if known_d.get(k, 0) >= c:
                                    continue
                                known_d[k] = c
                                engobj.wait_ge(dsem[k], c)
                        if ins.fn is None:
                            continue
                        r = ins.fn(engobj)
                        if ins.is_dma:
                            r.then_inc(dsem[ins.dsem], 16)
                        elif ins.signal:
                            r.then_inc(esem[e], 1)
                    if e == "sp":
                        for k, c in final_d.items():
                            if known_d.get(k, 0) < c:
                                engobj.wait_ge(dsem[k], c)
                return body

            block.tensor(make("pe"))
            block.scalar(make("act"))
            block.vector(make("dve"))
            block.gpsimd(make("pool"))
            block.sync(make("sp"))


class Rot:
    def __init__(self, tiles):
        self.tiles = tiles
        self.i = 0

    def next(self):
        t = self.tiles[self.i % len(self.tiles)]
        self.i += 1
        return t


def build_nc(debug=None, NPAIR_SB=4, NPAIR_DIL=2, NEXP_BLK=128, NTILE_PEER=8, NTT=16, NQT=8, NOWN_EFF=32):
    nc = bass.Bass("TRN2", target_bir_lowering=False)
    S = Sched(nc)

    def din(name, shape, dt=F32):
        return nc.dram_tensor(name, list(shape), dt, kind="ExternalInput").ap()

    xall = din("xall", [SEQ, D])
    cT = din("cT", [128, 8])
    kvalid_d = din("kvalid", [128, 1])
    ropeC_d = din("ropeC", [128, SEQ])
    ropeS_d = din("ropeS", [128, SEQ])
    w_ada = din("w_ada", [D, 6 * D])
    b_ada = din("b_ada", [1, 6 * D])
    gmix_d = din("g_mix", [1, D])
    gffn_d = din("g_ffn", [1, D])
    gfin_d = din("g_final", [1, D])
    w_in = din("w_in", [D, 5888])
    w_perm = din("w_perm", [D, 1536])
    w_sb_o = din("w_sb_o", [512, D])
    w_dil_o = din("w_dil_o", [256, D])
    w_out = din("w_out", [D, D])
    w_pq = din("w_pq", [D, 2048])
    keysT_d = din("keysT", [128, 16, 128])
    peer_u = din("peer_u", [16384, D])
    peer_v = din("peer_v", [16384, D])
    ident_d = din("ident", [128, 128])
    triI_d = din("tri_incl", [128, 128])
    triL_d = din("tri_low", [128, 128])
    dmask_d = din("diagmask", [128, 128])
    dilm_d = din("dilmask", [128, 24, 128])

    y_out = nc.dram_tensor("y", [NOWN * 128, D], F32, kind="ExternalOutput").ap()
    dbg_out = None
    if debug:
        dbg_out = nc.dram_tensor("dbg", [128, 32768], F32, kind="ExternalOutput").ap()

    def dscr(name, shape, dt):
        return nc.dram_tensor(name, list(shape), dt, kind="Internal").ap()

    hT_d = dscr("hT_scr", [16, 128, 8, 512], BF16)
    x1_d = dscr("x1_scr", [NOWN, 128, D], F32)
    uT_d = dscr("uT_scr", [128, 128, 8, 128], BF16)
    vB_d = dscr("vB_scr", [128, 128, D], BF16)
    rt_f_d = dscr("rt_f", [NOWN, 128, 2, 8, 128], F32)
    kap_d = dscr("kap", [NOWN, 128, 8], F32)
    h2T_d = dscr("h2T_scr", [NOWN, 128, 8, 128], BF16)

    w_in_r = w_in.rearrange("(k p) n -> p k n", p=128)
    w_perm_r = w_perm.rearrange("(k p) n -> p k n", p=128)

    top = contextlib.ExitStack()

    def T(es, name, shape, dt=F32):
        return es.enter_context(nc.sbuf_tensor(name, list(shape), dt))

    def P(es, name, shape, dt=F32):
        return es.enter_context(nc.psum_tensor(name, list(shape), dt))

    def mm(out, lhsT, rhs, start, stop, reads, writes):
        S.op("pe", lambda e: e.matmul(out, lhsT=lhsT, rhs=rhs, start=start, stop=stop),
             reads, writes)

    def tr(out, in_, ident, reads, writes):
        S.op("pe", lambda e: e.transpose(out=out, in_=in_, identity=ident), reads, writes)

    def act(out, in_, func, reads, writes, bias=None, scale=None, accum=None):
        def f(e):
            kw = {}
            if bias is not None:
                kw["bias"] = bias
            if scale is not None:
                kw["scale"] = scale
            if accum is not None:
                kw["accum_out"] = accum
            return e.activation(out=out, in_=in_, func=func, **kw)
        S.op("act", f, reads, writes)

    def tt(eng, out, in0, in1, op, reads, writes):
        S.op(eng, lambda e: e.tensor_tensor(out=out, in0=in0, in1=in1, op=op), reads, writes)

    def ts(eng, out, in0, s1, s2, op0, op1, reads, writes):
        if s2 is None:
            S.op(eng, lambda e: e.tensor_scalar(out=out, in0=in0, scalar1=s1, scalar2=None,
                                                op0=op0), reads, writes)
        else:
            S.op(eng, lambda e: e.tensor_scalar(out=out, in0=in0, scalar1=s1, scalar2=s2,
                                                op0=op0, op1=op1), reads, writes)

    def stt(eng, out, in0, scalar, in1, op0, op1, reads, writes):
        S.op(eng, lambda e: e.scalar_tensor_tensor(out=out, in0=in0, scalar=scalar, in1=in1,
                                                   op0=op0, op1=op1), reads, writes)

    def cp(eng, out, in_, reads, writes):
        S.op(eng, lambda e: e.tensor_copy(out=out, in_=in_), reads, writes)

    def dma(eng, out, in_, reads, writes, sem):
        S.dma(eng, lambda e: e.dma_start(out=out, in_=in_), reads, writes, sem)

    identb = T(top, "identb", [128, 128], BF16)
    identf = T(top, "identf", [128, 128], F32)
    onesb = T(top, "onesb", [128, 128], BF16)
    onesf = T(top, "onesf", [128, 128], F32)
    kval = T(top, "kval", [128, 1], F32)
    epsT = T(top, "epsT", [128, 1], F32)
    G2b = T(top, "G2b", [128, D])
    SH2b = T(top, "SH2b", [128, D])
    GT1b = T(top, "GT1b", [128, D])
    GT2b = T(top, "GT2b", [128, D])
    GFb = T(top, "GFb", [128, D])
    es_attn = contextlib.ExitStack()
    osbT = T(es_attn, "osbT", [128, 4, NOWN * 128], BF16)
    odT = T(es_attn, "odT", [128, 2, NOWN * 128], BF16)
    es01 = contextlib.ExitStack()
    G1b = T(es01, "G1b", [128, D])
    SH1b = T(es01, "SH1b", [128, D])

    with contextlib.ExitStack() as es:
        scT = T(es, "scT", [128, 8])
        scB = T(es, "scB", [128, 8, 128])
        wad = T(es, "wad", [128, 8, 1536])
        modb = T(es, "modb", [128, 6 * D])
        badab = T(es, "badab", [128, 6 * D])
        gtmp = T(es, "gtmp", [128, D])
        ps0 = [P(es, "ps0_%d" % i, [128, 512]) for i in range(3)]

        dma("sp", identf[:, :], ident_d, [], [identf], identf)
        dma("sp", kval[:, :], kvalid_d, [], [kval], kval)
        dma("sp", scT[:, :], cT, [], [scT], scT)
        dma("sp", badab[:, :], b_ada.partition_broadcast(128)[:, 0, :], [], [badab], badab)
        dma("sp", GFb[:, :], gfin_d.partition_broadcast(128)[:, 0, :], [], [GFb], GFb)
        cp("dve", identb[:, :], identf[:, :], [identf], [identb])
        S.op("dve", lambda e: e.memset(onesf[:, :], 1.0), [], [onesf])
        S.op("dve", lambda e: e.memset(onesb[:, :], 1.0), [], [onesb])
        S.op("dve", lambda e: e.memset(epsT[:, :], EPS), [], [epsT])
        act(scT[:, :], scT[:, :], AF.Silu, [scT], [scT])
        for kc in range(8):
            ts("dve", scB[:, kc, :], onesf[:, :], scT[:, kc:kc + 1], None, ALU.mult, None,
               [onesf, scT], [scB])
        w_ada_r = w_ada.rearrange("(k p) n -> p k n", p=128)
        for g in range(4):
            dma("sp", wad[:, :, :], w_ada_r[:, :, g * 1536:(g + 1) * 1536], [], [wad], wad)
            for j in range(3):
                for kc in range(8):
                    mm(ps0[j][:, :], scB[:, kc, :], wad[:, kc, j * 512:(j + 1) * 512],
                       kc == 0, kc == 7, [scB, wad], [ps0[j]])
                c0 = g * 1536 + j * 512
                tt("dve", modb[:, c0:c0 + 512], ps0[j][:, :], badab[:, c0:c0 + 512], ALU.add,
                   [ps0[j], badab], [modb])
        cp("dve", SH1b[:, :], modb[:, 0:D], [modb], [SH1b])
        cp("dve", GT1b[:, :], modb[:, 2 * D:3 * D], [modb], [GT1b])
        cp("dve", SH2b[:, :], modb[:, 3 * D:4 * D], [modb], [SH2b])
        cp("dve", GT2b[:, :], modb[:, 5 * D:6 * D], [modb], [GT2b])
        dma("sp", gtmp[:, :], gmix_d.partition_broadcast(128)[:, 0, :], [], [gtmp], gtmp)
        stt("dve", G1b[:, :], modb[:, D:2 * D], 1.0, gtmp[:, :], ALU.add, ALU.mult,
            [modb, gtmp], [G1b])
        dma("sp", gtmp[:, :], gffn_d.partition_broadcast(128)[:, 0, :], [G1b], [gtmp], gtmp)
        stt("dve", G2b[:, :], modb[:, 4 * D:5 * D], 1.0, gtmp[:, :], ALU.add, ALU.mult,
            [modb, gtmp], [G2b])
        S.barrier()

    def norm_mod_T(x_t, Gb, SHb, junk, ss, rstd, xn, hblk, pT, outT, res):
        act(junk[:, :], x_t[:, :], AF.Square, [x_t], [junk, ss], accum=ss[:, :])
        act(rstd[:, :], ss[:, :], AF.Sqrt, [ss, epsT], [rstd], bias=epsT[:, :], scale=1.0 / D)
        S.op("dve", lambda e: e.reciprocal(out=rstd[:, :], in_=rstd[:, :]), [rstd], [rstd])
        stt("dve", xn[:, :], x_t[:, :], rstd[:, 0:1], Gb[:, :], ALU.mult, ALU.mult,
            [x_t, rstd, Gb], [xn])
        tt("dve", hblk[:, :], xn[:, :], SHb[:, :], ALU.add, [xn, SHb], [hblk])
        for kc in range(8):
            tr(pT[:, kc, :], hblk[:, kc * 128:(kc + 1) * 128], identb[:, :],
               [hblk, identb], [pT])
        act(outT, pT[:, :, :], AF.Copy, [pT], [res])

    with contextlib.ExitStack() as es:
        xr = Rot([T(es, "p1x%d" % i, [128, D]) for i in range(3)])
        junk = T(es, "p1junk", [128, D], BF16)
        ssr = Rot([T(es, "p1ss%d" % i, [128, 1]) for i in range(2)])
        rsr = Rot([T(es, "p1rs%d" % i, [128, 1]) for i in range(2)])
        xnr = Rot([T(es, "p1xn%d" % i, [128, D]) for i in range(2)])
        hbr = Rot([T(es, "p1hb%d" % i, [128, D], BF16) for i in range(2)])
        hTr = Rot([T(es, "p1hT%d" % i, [128, 8, 512], BF16) for i in range(2)])
        pTr = Rot([P(es, "p1pT%d" % i, [128, 8, 128], BF16) for i in range(2)])
        for tt_i in range(NTT):
            hTt = hTr.next()
            for j in range(4):
                tb = tt_i * 4 + j
                x_t = xr.next()
                dma("sp", x_t[:, :], xall[tb * 128:(tb + 1) * 128, :], [], [x_t], x_t)
                norm_mod_T(x_t, G1b, SH1b, junk, ssr.next(), rsr.next(), xnr.next(), hbr.next(),
                           pTr.next(), hTt[:, :, j * 128:(j + 1) * 128], hTt)
            dma("pool", hT_d[tt_i], hTt[:, :, :], [hTt], [("hT", tt_i)], hTt)
        S.barrier()
    es01.close()

    def load_w_bf16(dst, src_ap, n, stage_rot, eng="dve", c_off=0):
        step = 128
        for c0 in range(0, n, step):
            st = stage_rot.next()
            dma("sp", st[:, :, :], src_ap[:, :, c0:c0 + step], [], [st], st)
            cp(eng, dst[:, :, c_off + c0:c_off + c0 + step], st[:, :, :], [st], [dst])

    with contextlib.ExitStack() as es:
        stg = Rot([T(es, "p2stg%d" % i, [128, 8, 128]) for i in range(2)])
        Wq = T(es, "p2Wq", [128, 8, 128], BF16)
        Wk = T(es, "p2Wk", [128, 8, 128], BF16)
        Wv = T(es, "p2Wv", [128, 8, 128], BF16)
        KT = T(es, "p2KT", [128, SEQ], BF16)
        V = T(es, "p2V", [128, NB, 128], BF16)
        QT = T(es, "p2QT", [128, NOWN * 128], BF16)
        hTr = Rot([T(es, "p2hT%d" % i, [128, 8, 512], BF16) for i in range(2)])
        triI = T(es, "p2triI", [128, 128], BF16)
        triL = T(es, "p2triL", [128, 128], BF16)
        dmk = T(es, "p2dmk", [128, 128], F32)
        tmpf = T(es, "p2tmpf", [128, 128], F32)
        Et = [Rot([T(es, "p2E%d_%d" % (h, i), [128, 512]) for i in range(2)]) for h in range(2)]
        Lt = [Rot([T(es, "p2L%d_%d" % (h, i), [128, 512], BF16) for i in range(2)]) for h in range(2)]
        EXt = [Rot([T(es, "p2X%d_%d" % (h, i), [128, 512]) for i in range(2)]) for h in range(2)]
        At = [Rot([T(es, "p2A%d_%d" % (h, i), [128, 512], BF16) for i in range(2)]) for h in range(2)]
        Zb4 = [P(es, "p2Z%d" % i, [128, 512]) for i in range(4)]
        Zp = [Rot(Zb4[0:2]), Rot(Zb4[2:4])]
        Cp = [P(es, "p2C%d" % h, [128, 512]) for h in range(2)]
        Op = [P(es, "p2O%d" % h, [128, 512]) for h in range(2)]
        Gp = Rot(Zb4)

        dma("sp", tmpf[:, :], triI_d, [], [tmpf], tmpf)
        cp("dve", triI[:, :], tmpf[:, :], [tmpf], [triI])
        dma("sp", tmpf[:, :], triL_d, [triI], [tmpf], tmpf)
        cp("dve", triL[:, :], tmpf[:, :], [tmpf], [triL])
        dma("sp", dmk[:, :], dmask_d, [], [dmk], dmk)

        if NPAIR_SB == 0:
            for pr in range(4):
                S.op("pool", lambda e, pr=pr: e.memset(osbT[:, pr, :], 0.0), [],
                     [("osbT", pr, q) for q in range(8)])
        for pair in range(NPAIR_SB):
            load_w_bf16(Wq, w_in_r[:, :, pair * 128:(pair + 1) * 128], 128, stg)
            load_w_bf16(Wk, w_in_r[:, :, 512 + pair * 128:512 + (pair + 1) * 128], 128, stg)
            load_w_bf16(Wv, w_in_r[:, :, 1024 + pair * 128:1024 + (pair + 1) * 128], 128, stg)
            for tt_i in range(NTT):
                hTt = hTr.next()
                dma("sp", hTt[:, :, :], hT_d[tt_i], [("hT", tt_i)], [hTt], hTt)
                g = Gp.next()
                for kc in range(8):
                    mm(g[:, :], Wk[:, kc, :], hTt[:, kc, :], kc == 0, kc == 7, [Wk, hTt], [g])
                cp("dve", KT[:, tt_i * 512:(tt_i + 1) * 512], g[:, :], [g], [("KT", tt_i)])
                g = Gp.next()
                for j in range(4):
                    for kc in range(8):
                        mm(g[:, j * 128:(j + 1) * 128], hTt[:, kc, j * 128:(j + 1) * 128],
                           Wv[:, kc, :], kc == 0, kc == 7, [Wv, hTt], [g])
                act(V[:, tt_i * 4:(tt_i + 1) * 4, :], g[:, :].rearrange("p (j c) -> p j c", j=4),
                    AF.Copy, [g], [("V", tt_i)])
                g = Gp.next()
                for jj, j in enumerate((1, 3)):
                    for kc in range(8):
                        mm(g[:, jj * 128:(jj + 1) * 128], Wq[:, kc, :],
                           hTt[:, kc, j * 128:(j + 1) * 128], kc == 0, kc == 7, [Wq, hTt], [g])
                n0 = tt_i * 2
                act(QT[:, n0 * 128:(n0 + 2) * 128], g[:, 0:256], AF.Copy, [g], [("QT", n0 // 4)],
                    scale=0.125)
            for qi in range(NQT):
                kb_top = 8 * qi + 7
                qres = ("QT", qi)
                st = {}

                def stage1_pe(kb, h):
                    jmin = max(0, (kb - 8 * qi) // 2)
                    c0 = jmin * 128
                    r0, r1 = h * 64, (h + 1) * 64
                    Z = Zp[h].next()
                    st[(kb, h)] = dict(c0=c0, Z=Z, diag=(kb == 8 * qi + 2 * jmin + 1))
                    mm(Z[:, c0:512], KT[r0:r1, kb * 128:(kb + 1) * 128],
                       QT[r0:r1, qi * 512 + c0:(qi + 1) * 512], True, True,
                       [("KT", kb // 4), qres], [Z])

                def stage1_el(kb, h):
                    d = st[(kb, h)]
                    c0, Z = d["c0"], d["Z"]
                    E = Et[h].next(); L = Lt[h].next()
                    d["E"], d["L"] = E, L
                    act(E[:, c0:512], Z[:, c0:512], AF.Exp, [Z], [E])
                    if d["diag"]:
                        tt("dve", E[:, c0:c0 + 128], E[:, c0:c0 + 128], dmk[:, :], ALU.mult,
                           [E, dmk], [E])
                    if kb == 0:
                        ts("dve", E[:, c0:512], E[:, c0:512], kval[:, 0:1], None, ALU.mult,
                           None, [E, kval], [E])
                    act(L[:, c0:512], E[:, c0:512], AF.Ln, [E], [L], bias=1.0)

                def c1(kb, h):
                    d = st[(kb, h)]
                    c0 = d["c0"]
                    mm(Cp[h][:, c0:512], triI[:, :], d["L"][:, c0:512], kb == kb_top, True,
                       [triI, d["L"]], [Cp[h]])

                def exa(kb, h):
                    d = st[(kb, h)]
                    c0 = d["c0"]
                    EX = EXt[h].next(); A = At[h].next()
                    d["A"] = A
                    act(EX[:, c0:512], Cp[h][:, c0:512], AF.Exp, [Cp[h]], [EX], scale=-1.0)
                    tt("dve", A[:, c0:512], d["E"][:, c0:512], EX[:, c0:512], ALU.mult,
                       [d["E"], EX], [A])

                def oc2(kb, h):
                    d = st[(kb, h)]
                    c0 = d["c0"]
                    mm(Op[h][:, c0:512], V[:, kb, :], d["A"][:, c0:512], kb == kb_top, kb == 0,
                       [("V", kb // 4), d["A"]], [Op[h]])
                    if kb > 0:
                        mm(Cp[h][:, c0:512], triL[:, :], d["L"][:, c0:512], False, False,
                           [triL, d["L"]], [Cp[h]])

                for h in range(2):
                    stage1_pe(kb_top, h)
                for h in range(2):
                    stage1_el(kb_top, h)
                for kb in range(kb_top, -1, -1):
                    for h in range(2):
                        c1(kb, h)
                    if kb > 0:
                        for h in range(2):
                            stage1_pe(kb - 1, h)
                    for h in range(2):
                        exa(kb, h)
                    if kb > 0:
                        for h in range(2):
                            stage1_el(kb - 1, h)
                    for h in range(2):
                        oc2(kb, h)
                for h in range(2):
                    r0, r1 = h * 64, (h + 1) * 64
                    act(osbT[r0:r1, pair, qi * 512:(qi + 1) * 512], Op[h][r0:r1, :], AF.Copy,
                        [Op[h]], [("osbT", pair, qi)])
        S.barrier()

    if debug == "sb":
        with contextlib.ExitStack() as es:
            st = Rot([T(es, "dbgst%d" % i, [128, 2048]) for i in range(2)])
            for pr in range(4):
                for c in range(2):
                    s_ = st.next()
                    cp("dve", s_[:, :], osbT[:, pr, c * 2048:(c + 1) * 2048],
                       [("osbT", pr, q) for q in range(8)], [s_])
                    dma("sp", dbg_out[:, pr * 4096 + c * 2048: pr * 4096 + (c + 1) * 2048], s_[:, :],
                        [s_], [], s_)
        es_attn.close(); top.close()
        S.emit()
        return nc

    with contextlib.ExitStack() as es:
        stg = Rot([T(es, "p3stg%d" % i, [128, 8, 128]) for i in range(2)])
        Wq = T(es, "p3Wq", [128, 8, 128], BF16)
        Wk = T(es, "p3Wk", [128, 8, 128], BF16)
        Wv = T(es, "p3Wv", [128, 8, 128], BF16)
        Wqp = T(es, "p3Wqp", [128, 8, 128], BF16)
        Wkp = T(es, "p3Wkp", [128, 8, 128], BF16)
        KT = T(es, "p3KT", [128, SEQ], BF16)
        V = T(es, "p3V", [128, NB, 128], BF16)
        QT = T(es, "p3QT", [128, NOWN * 256], BF16)
        hTr = Rot([T(es, "p3hT%d" % i, [128, 8, 512], BF16) for i in range(2)])
        Cr = Rot([T(es, "p3C%d" % i, [128, 512]) for i in range(2)])
        Sr = Rot([T(es, "p3S%d" % i, [128, 512]) for i in range(2)])
        t1r = Rot([T(es, "p3t1%d" % i, [128, 512]) for i in range(2)])
        t2r = Rot([T(es, "p3t2%d" % i, [128, 512]) for i in range(2)])
        dilm = T(es, "p3dilm", [128, 24, 128], BF16)
        Er = Rot([T(es, "p3E%d" % i, [128, 256], BF16) for i in range(3)])
        Oacc = T(es, "p3Oacc", [128, NOWN * 128], F32)
        Zacc = T(es, "p3Zacc", [128, NOWN * 128], F32)
        Gp = Rot([P(es, "p3G%d" % i, [128, 512]) for i in range(4)])
        Zr = Rot([P(es, "p3Z%d" % i, [128, 512]) for i in range(2)])
        Ob = P(es, "p3Ob", [128, 512])
        Zs = P(es, "p3Zs", [128, 512])

        for m0 in range(0, 24, 8):
            st = stg.next()
            dma("sp", st[:, :, :], dilm_d[:, m0:m0 + 8, :], [], [st], st)
            cp("dve", dilm[:, m0:m0 + 8, :], st[:, :, :], [st], [dilm])
        mbase = (0, 2, 7)
        S.op("pool", lambda e: e.memset(QT[:, :], 0.0), [], [("QT", q) for q in range(8)])
        PENG = "dve" if "p" in DBG_SKIP else "pool"
        for sp_i in range(NPAIR_DIL):
            for g, (r, nkb) in enumerate(DIL):
                hc = (g * 4 + 2 * sp_i) * 64
                load_w_bf16(Wq, w_in_r[:, :, 1536 + hc:1536 + hc + 128], 128, stg)
                load_w_bf16(Wk, w_in_r[:, :, 2304 + hc:2304 + hc + 128], 128, stg)
                load_w_bf16(Wv, w_in_r[:, :, 3072 + hc:3072 + hc + 128], 128, stg)
                load_w_bf16(Wqp, w_perm_r[:, :, hc:hc + 128], 128, stg)
                load_w_bf16(Wkp, w_perm_r[:, :, 768 + hc:768 + hc + 128], 128, stg)
                for tt_i in range(NTT):
                    hTt = hTr.next(); Ct = Cr.next(); St = Sr.next()
                    dma("sp", hTt[:, :, :], hT_d[tt_i], [("hT", tt_i)], [hTt], hTt)
                    dma("sp", Ct[:, :], ropeC_d[:, tt_i * 512:(tt_i + 1) * 512], [], [Ct], Ct)
                    dma("sp", St[:, :], ropeS_d[:, tt_i * 512:(tt_i + 1) * 512], [], [St], St)
                    g1 = Gp.next(); g2 = Gp.next(); t1 = t1r.next(); t2 = t2r.next()
                    for kc in range(8):
                        mm(g1[:, :], Wk[:, kc, :], hTt[:, kc, :], kc == 0, kc == 7, [Wk, hTt], [g1])
                    for kc in range(8):
                        mm(g2[:, :], Wkp[:, kc, :], hTt[:, kc, :], kc == 0, kc == 7, [Wkp, hTt], [g2])
                    tt("dve", t1[:, :], g1[:, :], Ct[:, :], ALU.mult, [g1, Ct], [t1])
                    tt("dve", t2[:, :], g2[:, :], St[:, :], ALU.mult, [g2, St], [t2])
                    tt(PENG, KT[:, tt_i * 512:(tt_i + 1) * 512], t1[:, :], t2[:, :], ALU.add,
                       [t1, t2], [("KT", tt_i)])
                    g3 = Gp.next()
                    for j in range(4):
                        for kc in range(8):
                            mm(g3[:, j * 128:(j + 1) * 128], hTt[:, kc, j * 128:(j + 1) * 128],
                               Wv[:, kc, :], kc == 0, kc == 7, [Wv, hTt], [g3])
                    act(V[:, tt_i * 4:(tt_i + 1) * 4, :], g3[:, :].rearrange("p (j c) -> p j c", j=4),
                        AF.Copy, [g3], [("V", tt_i)])
                    g1 = Gp.next(); g2 = Gp.next(); t1 = t1r.next(); t2 = t2r.next()
                    for jj, j in enumerate((1, 3)):
                        for kc in range(8):
                            mm(g1[:, jj * 128:(jj + 1) * 128], Wq[:, kc, :],
                               hTt[:, kc, j * 128:(j + 1) * 128], kc == 0, kc == 7, [Wq, hTt], [g1])
                        for kc in range(8):
                            mm(g2[:, jj * 128:(jj + 1) * 128], Wqp[:, kc, :],
                               hTt[:, kc, j * 128:(j + 1) * 128], kc == 0, kc == 7, [Wqp, hTt], [g2])
                    Cv = Ct[:, :].rearrange("p (j c) -> p j c", j=4)[:, 1::2, :]
                    Sv = St[:, :].rearrange("p (j c) -> p j c", j=4)[:, 1::2, :]
                    v3 = lambda ap: ap.rearrange("p (j c) -> p j c", j=2)
                    tt("dve", v3(t1[:, 0:256]), v3(g1[:, 0:256]), Cv, ALU.mult, [g1, Ct], [t1])
                    tt("dve", v3(t2[:, 0:256]), v3(g2[:, 0:256]), Sv, ALU.mult, [g2, St], [t2])
                    n0 = tt_i * 2
                    for h in range(2):
                        r0, r1 = h * 64, (h + 1) * 64
                        qv = QT[r0:r1, n0 * 256:(n0 + 2) * 256].rearrange(
                            "p (j c) -> p j c", j=2)[:, :, h * 128:(h + 1) * 128]
                        tt(PENG, qv, v3(t1[r0:r1, 0:256]), v3(t2[r0:r1, 0:256]), ALU.add,
                           [t1, t2], [("QT", n0 // 4)])
                flat = []
                for n in range(0 if "a" in DBG_SKIP else NOWN_EFF):
                    Pb = 2 * n + 1
                    steps = [dl for dl in range(nkb) if Pb - dl >= 0]
                    for si, dl in enumerate(steps):
                        flat.append((n, si, dl, len(steps)))
                stA = {}

                def stageA(k):
                    n, si, dl, ns = flat[k]
                    kb = 2 * n + 1 - dl
                    Zb = Zr.next(); E = Er.next()
                    mm(Zb[:, 0:256], KT[:, kb * 128:(kb + 1) * 128],
                       QT[:, n * 256:(n + 1) * 256], True, True,
                       [("KT", kb // 4), ("QT", n // 4)], [Zb])
                    act(E[:, :], Zb[:, 0:256], AF.Exp, [Zb], [E], scale=0.125)
                    mi = mbase[g] + dl
                    tt(PENG, E[:, :].rearrange("p (h q) -> p h q", h=2),
                       E[:, :].rearrange("p (h q) -> p h q", h=2),
                       dilm[:, mi:mi + 1, :].to_broadcast([128, 2, 128]), ALU.mult,
                       [E, dilm], [E])
                    if kb == 0:
                        ts("dve", E[:, :], E[:, :], kval[:, 0:1], None, ALU.mult, None,
                           [E, kval], [E])
                    stA[k] = E

                def stageB(k):
                    n, si, dl, ns = flat[k]
                    kb = 2 * n + 1 - dl
                    E = stA.pop(k)
                    mm(Ob[:, 0:256], V[:, kb, :], E[:, :], si == 0, si == ns - 1,
                       [("V", kb // 4), E], [Ob])
                    mm(Zs[:, 0:256], onesb[:, :], E[:, :], si == 0, si == ns - 1,
                       [onesb, E], [Zs])
                    if si == ns - 1:
                        for h in range(2):
                            r0, r1 = h * 64, (h + 1) * 64
                            oa = Oacc[r0:r1, n * 128:(n + 1) * 128]
                            za = Zacc[r0:r1, n * 128:(n + 1) * 128]
                            if g == 0:
                                cp("dve", oa, Ob[r0:r1, h * 128:(h + 1) * 128], [Ob], [Oacc])
                                cp("dve", za, Zs[r0:r1, h * 128:(h + 1) * 128], [Zs], [Zacc])
                            else:
                                tt("dve", oa, Ob[r0:r1, h * 128:(h + 1) * 128], oa, ALU.add,
                                   [Ob, Oacc], [Oacc])
                                tt("dve", za, Zs[r0:r1, h * 128:(h + 1) * 128], za, ALU.add,
                                   [Zs, Zacc], [Zacc])

                if flat:
                    stageA(0)
                for k in range(len(flat)):
                    if k + 1 < len(flat):
                        stageA(k + 1)
                    stageB(k)
            NE = NOWN_EFF * 128
            S.op("dve", lambda e: e.reciprocal(out=Zacc[:, 0:NE], in_=Zacc[:, 0:NE]), [Zacc], [Zacc])
            tt("dve", odT[:, sp_i, 0:NE], Oacc[:, 0:NE], Zacc[:, 0:NE], ALU.mult, [Oacc, Zacc],
               [("odT", sp_i)])
        S.barrier()

    if debug == "dil":
        with contextlib.ExitStack() as es:
            st = Rot([T(es, "dbgst%d" % i, [128, 2048]) for i in range(2)])
            for pr in range(2):
                for c in range(2):
                    s_ = st.next()
                    cp("dve", s_[:, :], odT[:, pr, c * 2048:(c + 1) * 2048], [("odT", pr)], [s_])
                    dma("sp", dbg_out[:, pr * 4096 + c * 2048: pr * 4096 + (c + 1) * 2048], s_[:, :],
                        [s_], [], s_)
        es_attn.close(); top.close()
        S.emit()
        return nc

    with contextlib.ExitStack() as es:
        stg = Rot([T(es, "p4stg%d" % i, [128, 8, 128]) for i in range(2)])
        Wg = T(es, "p4Wg", [128, 8, 2048], BF16)
        Wsbo = T(es, "p4Wsbo", [128, 4, 1024], BF16)
        Wdo = T(es, "p4Wdo", [128, 2, 1024], BF16)
        Wout = T(es, "p4Wout", [128, 8, 1024], BF16)
        hBr = Rot([T(es, "p4hB%d" % i, [128, 8, 128], BF16) for i in range(2)])
        sg = T(es, "p4sg", [128, 2048], BF16)
        m1 = T(es, "p4m1", [128, D])
        m2 = T(es, "p4m2", [128, D])
        mg = T(es, "p4mg", [128, D], BF16)
        mT = T(es, "p4mT", [128, 8, 128], BF16)
        xr = Rot([T(es, "p4x%d" % i, [128, D]) for i in range(2)])
        x1r = Rot([T(es, "p4x1%d" % i, [128, D]) for i in range(2)])
        tmpr = T(es, "p4tmp", [128, D])
        pg = [P(es, "p4pg%d" % i, [128, 512]) for i in range(4)]
        pbs = [P(es, "p4pbs%d" % i, [128, 512]) for i in range(2)]
        pbd = [P(es, "p4pbd%d" % i, [128, 512]) for i in range(2)]

        load_w_bf16(Wg, w_in_r[:, :, 3840:5888], 2048, stg)
        for t4 in range(4):
            for c0 in range(0, 1024, 512):
                st = stg.next()
                stv = st[:, :, :].rearrange("p a b -> p (a b)")
                dma("sp", stv[:, 0:512], w_sb_o[t4 * 128:(t4 + 1) * 128, c0:c0 + 512], [], [st], st)
                cp("dve", Wsbo[:, t4, c0:c0 + 512], stv[:, 0:512], [st], [Wsbo])
        for t2 in range(2):
            for c0 in range(0, 1024, 512):
                st = stg.next()
                stv = st[:, :, :].rearrange("p a b -> p (a b)")
                dma("sp", stv[:, 0:512], w_dil_o[t2 * 128:(t2 + 1) * 128, c0:c0 + 512], [], [st], st)
                cp("dve", Wdo[:, t2, c0:c0 + 512], stv[:, 0:512], [st], [Wdo])
        load_w_bf16(Wout, w_out.rearrange("(k p) n -> p k n", p=128), 1024, stg)

        for n in range(NOWN_EFF):
            tti, j = n // 2, 1 + 2 * (n % 2)
            hB = hBr.next()
            dma("sp", hB[:, :, :], hT_d[tti][:, :, j * 128:(j + 1) * 128], [("hT", tti)], [hB], hB)
            for c4 in range(4):
                for kc in range(8):
                    mm(pg[c4][:, :], hB[:, kc, :], Wg[:, kc, c4 * 512:(c4 + 1) * 512],
                       kc == 0, kc == 7, [hB, Wg], [pg[c4]])
                act(sg[:, c4 * 512:(c4 + 1) * 512], pg[c4][:, :], AF.Sigmoid, [pg[c4]], [sg])
            for nh in range(2):
                for t4 in range(4):
                    mm(pbs[nh][:, :], osbT[:, t4, n * 128:(n + 1) * 128],
                       Wsbo[:, t4, nh * 512:(nh + 1) * 512], t4 == 0, t4 == 3,
                       [("osbT", t4, n // 4), Wsbo], [pbs[nh]])
                for t2 in range(2):
                    mm(pbd[nh][:, :], odT[:, t2, n * 128:(n + 1) * 128],
                       Wdo[:, t2, nh * 512:(nh + 1) * 512], t2 == 0, t2 == 1,
                       [("odT", t2), Wdo], [pbd[nh]])
                tt("dve", m1[:, nh * 512:(nh + 1) * 512], pbs[nh][:, :],
                   sg[:, nh * 512:(nh + 1) * 512], ALU.mult, [pbs[nh], sg], [m1])
                tt("dve", m2[:, nh * 512:(nh + 1) * 512], pbd[nh][:, :],
                   sg[:, 1024 + nh * 512:1024 + (nh + 1) * 512], ALU.mult, [pbd[nh], sg], [m2])
            tt("pool", mg[:, :], m1[:, :], m2[:, :], ALU.add, [m1, m2], [mg])
            pTm = pg[0][:, :].bitcast(BF16).rearrange("p (k t) -> p k t", k=8)
            for kc in range(8):
                tr(pTm[:, kc, :], mg[:, kc * 128:(kc + 1) * 128], identb[:, :], [mg, identb], [pg[0]])
            act(mT[:, :, :], pTm, AF.Copy, [pg[0]], [mT])
            x_t = xr.next(); x1 = x1r.next()
            tb = 2 * n + 1
            dma("sp", x_t[:, :], xall[tb * 128:(tb + 1) * 128, :], [], [x_t], x_t)
            for nh in range(2):
                for kc in range(8):
                    mm(pg[1 + nh][:, :], mT[:, kc, :], Wout[:, kc, nh * 512:(nh + 1) * 512],
                       kc == 0, kc == 7, [mT, Wout], [pg[1 + nh]])
                tt("dve", tmpr[:, nh * 512:(nh + 1) * 512], pg[1 + nh][:, :],
                   GT1b[:, nh * 512:(nh + 1) * 512], ALU.mult, [pg[1 + nh], GT1b], [tmpr])
            tt("pool", x1[:, :], tmpr[:, :], x_t[:, :], ALU.add, [tmpr, x_t], [x1])
            dma("pool", x1_d[n], x1[:, :], [x1], [("x1", n)], x1)
            if debug == "x1":
                dma("pool", dbg_out[:, n * 1024:(n + 1) * 1024], x1[:, :], [x1], [], x1)
        S.barrier()
    es_attn.close()

    if debug == "x1":
        top.close()
        S.emit()
        return nc

    NEG = -1.0e30
    with contextlib.ExitStack() as es:
        stg = Rot([T(es, "p5stg%d" % i, [128, 8, 128]) for i in range(2)])
        Wpq = T(es, "p5Wpq", [128, 8, 2048], BF16)
        keysf = T(es, "p5keysf", [128, 16, 128], F32)
        keysb = T(es, "p5keysb", [128, 16, 128], BF16)
        xr = Rot([T(es, "p5x%d" % i, [128, D]) for i in range(2)])
        junk = T(es, "p5junk", [128, D], BF16)
        ss = T(es, "p5ss", [128, 1]); rstd = T(es, "p5rstd", [128, 1])
        xn = T(es, "p5xn", [128, D]); hblk = T(es, "p5hblk", [128, D], BF16)
        h2Br = Rot([T(es, "p5h2B%d" % i, [128, 8, 128], BF16) for i in range(2)])
        qT = T(es, "p5qT", [128, 16, 128], BF16)
        s_all = T(es, "p5sall", [128, 16, 128], F32)
        work = T(es, "p5work", [128, 16, 128], F32)
        top16 = T(es, "p5top16", [128, 16, 16], F32)
        cand = T(es, "p5cand", [128, 8, 256], F32)
        cw1 = T(es, "p5cw1", [128, 8, 256], F32)
        cw2 = T(es, "p5cw2", [128, 8, 256], F32)
        c24 = T(es, "p5c24", [128, 8, 24], F32)
        tau = T(es, "p5tau", [128, 8], F32)
        zt = T(es, "p5zt", [128, 8, 16], F32)
        Zsum = T(es, "p5Zsum", [128, 8], F32)
        d0 = T(es, "p5d0", [128, 8, 128], F32)
        e1 = T(es, "p5e1", [128, 8, 128], F32)
        rfr = Rot([T(es, "p5rf%d" % i, [128, 2, 8, 128], F32) for i in range(2)])
        tm8 = T(es, "p5tm8", [128, 8], F32)
        kpr = Rot([T(es, "p5kp%d" % i, [128, 8], F32) for i in range(2)])
        pT = P(es, "p5pT", [128, 8, 128], BF16)
        pq = [P(es, "p5pq%d" % i, [128, 512]) for i in range(4)]

        load_w_bf16(Wpq, w_pq.rearrange("(k p) n -> p k n", p=128), 2048, stg)
        dma("sp", keysf[:, :, :], keysT_d, [], [keysf], keysf)
        cp("dve", keysb[:, :, :], keysf[:, :, :], [keysf], [keysb])

        for n in range(NOWN_EFF):
            x_t = xr.next(); h2B = h2Br.next(); rf = rfr.next(); kp = kpr.next()
            dma("sp", x_t[:, :], x1_d[n], [("x1", n)], [x_t], x_t)
            norm_mod_T(x_t, G2b, SH2b, junk, ss, rstd, xn, hblk, pT, h2B[:, :, :], h2B)
            dma("pool", h2T_d[n], h2B[:, :, :], [h2B], [("h2T", n)], h2B)
            for hp in range(16):
                for kc in range(8):
                    mm(pq[hp // 4][:, (hp % 4) * 128:(hp % 4 + 1) * 128],
                       Wpq[:, kc, hp * 128:(hp + 1) * 128], h2B[:, kc, :], kc == 0, kc == 7,
                       [Wpq, h2B], [pq[hp // 4]])
            for b4 in range(4):
                act(qT[:, b4 * 4:(b4 + 1) * 4, :],
                    pq[b4][:, :].rearrange("p (a c) -> p a c", a=4), AF.Copy, [pq[b4]], [qT])
            for hp in range(16):
                mm(pq[hp // 4][:, (hp % 4) * 128:(hp % 4 + 1) * 128], qT[:, hp, :], keysb[:, hp, :],
                   True, True, [qT, keysb], [pq[hp // 4]])
            for b4 in range(4):
                cp("dve", s_all[:, b4 * 4:(b4 + 1) * 4, :],
                   pq[b4][:, :].rearrange("p (a c) -> p a c", a=4), [pq[b4]], [s_all])
            for hp in range(16):
                S.op("dve", lambda e, hp=hp: e.max(out=top16[:, hp, 0:8], in_=s_all[:, hp, :]),
                     [s_all], [top16])
                S.op("dve", lambda e, hp=hp: e.match_replace(
                    out=work[:, hp, :], in_to_replace=top16[:, hp, 0:8], in_values=s_all[:, hp, :],
                    imm_value=NEG), [s_all, top16], [work])
                S.op("dve", lambda e, hp=hp: e.max(out=top16[:, hp, 8:16], in_=work[:, hp, :]),
                     [work], [top16])
            t4v = top16[:, :, :].rearrange("p (h two) k -> p h two k", two=2)
            a0 = t4v[:, :, 0, :].unsqueeze(3).to_broadcast([128, 8, 16, 16])
            a1 = t4v[:, :, 1, :].unsqueeze(2).to_broadcast([128, 8, 16, 16])
            tt("dve", cand[:, :, :].rearrange("p h (a b) -> p h a b", a=16), a0, a1, ALU.add,
               [top16], [cand])
            for h in range(8):
                S.op("dve", lambda e, h=h: e.max(out=c24[:, h, 0:8], in_=cand[:, h, :]), [cand], [c24])
                S.op("dve", lambda e, h=h: e.match_replace(
                    out=cw1[:, h, :], in_to_replace=c24[:, h, 0:8], in_values=cand[:, h, :],
                    imm_value=NEG), [cand, c24], [cw1])
                S.op("dve", lambda e, h=h: e.max(out=c24[:, h, 8:16], in_=cw1[:, h, :]), [cw1], [c24])
                S.op("dve", lambda e, h=h: e.match_replace(
                    out=cw2[:, h, :], in_to_replace=c24[:, h, 8:16], in_values=cw1[:, h, :],
                    imm_value=NEG), [cw1, c24], [cw2])
                S.op("dve", lambda e, h=h: e.max(out=c24[:, h, 16:24], in_=cw2[:, h, :]), [cw2], [c24])
            tt("dve", tau[:, :], c24[:, :, 15], c24[:, :, 16], ALU.add, [c24], [tau])
            ts("dve", tau[:, :], tau[:, :], 0.5, None, ALU.mult, None, [tau], [tau])
            tt("dve", zt[:, :, :], c24[:, :, 0:16], c24[:, :, 0:1].to_broadcast([128, 8, 16]),
               ALU.subtract, [c24], [zt])
            act(zt[:, :, :], zt[:, :, :], AF.Exp, [zt], [zt])
            S.op("dve", lambda e: e.reduce_sum(out=Zsum[:, :], in_=zt[:, :, :], axis=AX.X),
                 [zt], [Zsum])
            S.op("dve", lambda e: e.reciprocal(out=Zsum[:, :], in_=Zsum[:, :]), [Zsum], [Zsum])
            s4 = s_all[:, :, :].rearrange("p (h two) j -> p h two j", two=2)
            s0v, s1v = s4[:, :, 0, :], s4[:, :, 1, :]
            m0v = t4v[:, :, 0, 0:1].to_broadcast([128, 8, 128])
            m1v = t4v[:, :, 1, 0:1].to_broadcast([128, 8, 128])
            tt("dve", d0[:, :, :], s1v, m1v, ALU.subtract, [s_all, top16], [d0])
            act(rf[:, 0, :, :], d0[:, :, :], AF.Exp, [d0], [rf])
            tt("dve", tm8[:, :], t4v[:, :, 1, 0], tau[:, :], ALU.subtract, [top16, tau], [tm8])
            tt("dve", e1[:, :, :], s0v, tm8[:, :].unsqueeze(2).to_broadcast([128, 8, 128]), ALU.add,
               [s_all, tm8], [e1])
            act(rf[:, 1, :, :], e1[:, :, :], AF.Exp, [e1], [rf])
            tt("dve", kp[:, :], tau[:, :], c24[:, :, 0], ALU.subtract, [tau, c24], [kp])
            act(kp[:, :], kp[:, :], AF.Exp, [kp], [kp])
            tt("dve", kp[:, :], kp[:, :], Zsum[:, :], ALU.mult, [kp, Zsum], [kp])
            dma("pool", rt_f_d[n], rf[:, :, :, :], [rf], [("rtf", n)], rf)
            dma("pool", kap_d[n], kp[:, :], [kp], [("kap", n)], kp)
        S.barrier()

    if debug == "rt":
        top.close()
        S.emit()
        return nc

    with contextlib.ExitStack() as es:
        ur = Rot([T(es, "p6u%d" % i, [128, D]) for i in range(2)])
        vr = Rot([T(es, "p6v%d" % i, [128, D]) for i in range(2)])
        ubr = Rot([T(es, "p6ub%d" % i, [128, D], BF16) for i in range(2)])
        vbr = Rot([T(es, "p6vb%d" % i, [128, D], BF16) for i in range(2)])
        uTr = Rot([T(es, "p6uT%d" % i, [128, 8, 128], BF16) for i in range(2)])
        pTr = Rot([P(es, "p6pT%d" % i, [128, 8, 128], BF16) for i in range(2)])
        for i in range(NEXP_BLK):
            u_t = ur.next(); v_t = vr.next(); ub = ubr.next(); vb = vbr.next(); uT = uTr.next()
            pT = pTr.next()
            dma("sp", u_t[:, :], peer_u[i * 128:(i + 1) * 128, :], [], [u_t], u_t)
            dma("sp", v_t[:, :], peer_v[i * 128:(i + 1) * 128, :], [], [v_t], v_t)
            act(ub[:, :], u_t[:, :], AF.Copy, [u_t], [ub])
            for kc in range(8):
                tr(pT[:, kc, :], ub[:, kc * 128:(kc + 1) * 128], identb[:, :], [ub, identb], [pT])
            cp("dve", uT[:, :, :], pT[:, :, :], [pT], [uT])
            dma("pool", uT_d[i], uT[:, :, :], [uT], [("uT", i)], uT)
            cp("dve", vb[:, :], v_t[:, :], [v_t], [vb])
            dma("pool", vB_d[i], vb[:, :], [vb], [("vB", i)], vb)
        S.barrier()

    if debug == "5a":
        top.close()
        S.emit()
        return nc

    IG = 4
    with contextlib.ExitStack() as es:
        h2t = T(es, "p7h2t", [128, 8, 512], BF16)
        rf = T(es, "p7rf", [128, 4, 2, 8, 128], F32)
        kp = T(es, "p7kp", [128, 4, 8], F32)
        dg = T(es, "p7dg", [128, 4, 8, 128], BF16)
        yacc = T(es, "p7yacc", [128, 4, D], F32)
        uTg_r = Rot([T(es, "p7uT%d" % i, [128, IG, 1024], BF16) for i in range(2)])
        vBg_r = Rot([T(es, "p7vB%d" % i, [128, IG, 1024], BF16) for i in range(2)])
        ga2_r = Rot([T(es, "p7ga2%d" % i, [128, IG, 512], BF16) for i in range(2)])
        eD_r = Rot([T(es, "p7eD%d" % i, [128, 4, 8, 128], F32) for i in range(2)])
        r1_r = Rot([T(es, "p7r1%d" % i, [128, 4, 8, 128], BF16) for i in range(2)])
        ga_r = Rot([T(es, "p7ga%d" % i, [128, 512]) for i in range(2)])
        x1t = T(es, "p7x1", [128, D]); x2 = T(es, "p7x2", [128, D])
        junk = T(es, "p7junk", [128, D], BF16)
        ss = T(es, "p7ss", [128, 1]); rstd = T(es, "p7rstd", [128, 1])
        yo = Rot([T(es, "p7yo%d" % i, [128, D]) for i in range(2)])
        Pa = Rot([P(es, "p7pa%d" % i, [128, 512]) for i in range(2)])
        Pg = Rot([P(es, "p7pg%d" % i, [128, 512]) for i in range(2)])
        Py = Rot([P(es, "p7py%d" % i, [128, 1024]) for i in range(2)])

        for tk in range(NTILE_PEER):
            for b in range(4):
                n = 4 * tk + b
                dma("sp", h2t[:, :, b * 128:(b + 1) * 128], h2T_d[n], [("h2T", n)], [h2t], h2t)
                dma("sp", rf[:, b, :, :, :], rt_f_d[n], [("rtf", n)], [rf], rf)
                dma("sp", kp[:, b, :], kap_d[n], [("kap", n)], [kp], kp)
            for b in range(4):
                for h in range(8):
                    ts("dve", dg[:, b, h, :], identb[:, :], kp[:, b, h:h + 1], None, ALU.mult, None,
                       [identb, kp], [dg])
            NI = NEXP_BLK
            grp = {}

            def get_group(ig):
                if ig not in grp:
                    uTg = uTg_r.next(); vBg = vBg_r.next(); ga2 = ga2_r.next()
                    i0 = ig * IG
                    dma("sp", uTg[:, :, :], uT_d[i0:i0 + IG].rearrange("i d k e -> d i (k e)"),
                        [("uT", i) for i in range(i0, i0 + IG)], [uTg], uTg)
                    dma("sp", vBg[:, :, :], vB_d[i0:i0 + IG].rearrange("i e d -> e i d"),
                        [("vB", i) for i in range(i0, i0 + IG)], [vBg], vBg)
                    grp[ig] = (uTg, vBg, ga2)
                return grp[ig]

            stU = {}

            def emit_U(i):
                uTg, vBg, ga2 = get_group(i // IG)
                ii = i % IG
                pa = Pa.next(); ga = ga_r.next()
                for kc in range(8):
                    mm(pa[:, :], uTg[:, ii, kc * 128:(kc + 1) * 128], h2t[:, kc, :],
                       kc == 0, kc == 7, [uTg, h2t], [pa])
                act(ga[:, :], pa[:, :], AF.Gelu_apprx_tanh, [pa], [ga])
                stU[i] = ga

            def emit_gate(i):
                eD = eD_r.next(); r1 = r1_r.next()
                slot = id(eD)
                keys = []
                for b in range(4):
                    for h in range(8):
                        kk = ("eD", slot, b, h)
                        keys.append(kk)
                        act(eD[:, b, h, :], rf[:, b, 0, h, :], AF.Copy, [rf], [kk],
                            scale=rf[:, b, 1, h, i:i + 1])
                stt("dve", r1[:, :, :, :], eD[:, :, :, :], 1.0, eD[:, :, :, :], ALU.is_ge,
                    ALU.mult, keys, [r1])
                return r1

            def emit_diag(i, r1):
                uTg, vBg, ga2 = get_group(i // IG)
                ii = i % IG
                pgt = Pg.next()
                for b in range(4):
                    for h in range(8):
                        mm(pgt[:, b * 128:(b + 1) * 128], r1[:, b, h, :], dg[:, b, h, :],
                           h == 0, h == 7, [r1, dg], [pgt])
                ga = stU.pop(i)
                tt("dve", ga2[:, ii, :], pgt[:, :], ga[:, :], ALU.mult, [pgt, ga], [ga2])

            def emit_V(ig):
                uTg, vBg, ga2 = grp.pop(ig)
                for b in range(4):
                    py = Py.next()
                    for ii in range(IG):
                        for nh in range(2):
                            mm(py[:, nh * 512:(nh + 1) * 512], ga2[:, ii, b * 128:(b + 1) * 128],
                               vBg[:, ii, nh * 512:(nh + 1) * 512], ii == 0, ii == IG - 1,
                               [ga2, vBg], [py])
                    if ig == 0:
                        cp("dve", yacc[:, b, :], py[:, :], [py], [yacc])
                    else:
                        tt("dve", yacc[:, b, :], py[:, :], yacc[:, b, :], ALU.add, [py, yacc], [yacc])

            emit_U(0)
            for i in range(NI):
                r1 = emit_gate(i)
                if i + 1 < NI:
                    emit_U(i + 1)
                emit_diag(i, r1)
                if i % IG == IG - 1:
                    emit_V(i // IG)
            for b in range(4):
                n = 4 * tk + b
                y_t = yo.next()
                dma("sp", x1t[:, :], x1_d[n], [("x1", n)], [x1t], x1t)
                tt("dve", x2[:, :], yacc[:, b, :], GT2b[:, :], ALU.mult, [yacc, GT2b], [x2])
                tt("dve", x2[:, :], x2[:, :], x1t[:, :], ALU.add, [x2, x1t], [x2])
                act(junk[:, :], x2[:, :], AF.Square, [x2], [junk, ss], accum=ss[:, :])
                act(rstd[:, :], ss[:, :], AF.Sqrt, [ss, epsT], [rstd], bias=epsT[:, :], scale=1.0 / D)
                S.op("dve", lambda e: e.reciprocal(out=rstd[:, :], in_=rstd[:, :]), [rstd], [rstd])
                stt("dve", y_t[:, :], x2[:, :], rstd[:, 0:1], GFb[:, :], ALU.mult, ALU.mult,
                    [x2, rstd, GFb], [y_t])
                dma("pool", y_out[n * 128:(n + 1) * 128, :], y_t[:, :], [y_t], [], y_t)

    top.close()
    build_nc.stats = {e: len(S.q[e]) for e in ENGS}
    S.emit()
    return nc


def _consts():
    a = np.arange(128)
    ident = np.eye(128, dtype=np.float32)
    tri_incl = (a[:, None] >= a[None, :]).astype(np.float32)
    tri_low = (a[:, None] < a[None, :]).astype(np.float32)
    diagmask = (a[:, None] < a[None, :]).astype(np.float32)
    masks = []
    for r, nkb in DIL:
        for dl in range(nkb):
            d = 128 * dl + a[None, :] - a[:, None]
            masks.append(((d >= 0) & (d % r == 0) & (d <= 128 * r)).astype(np.float32))
    dilmask = np.stack(masks, axis=1)
    return ident, tri_incl, tri_low, diagmask, np.ascontiguousarray(dilmask)


def _rope_tables(shift):
    half = 32
    inv = (10000.0 ** (-np.arange(half, dtype=np.float32) / half)).astype(np.float32)
    pos = (np.arange(SEQ) - shift).astype(np.float32)
    ang = pos[:, None] * inv[None, :]
    cos = np.cos(ang).astype(np.float32).T
    sin = np.sin(ang).astype(np.float32).T
    C64 = np.concatenate([cos, cos], axis=0)
    S64 = np.concatenate([-sin, sin], axis=0)
    return (np.ascontiguousarray(np.concatenate([C64, C64], axis=0)),
            np.ascontiguousarray(np.concatenate([S64, S64], axis=0)))


def make_in_maps(x, c, w_ada, b_ada, g_mix, w_in, w_sb_o, w_dil_o, w_out, g_ffn, w_pq,
                 peer_keys, peer_u, peer_v, g_final):
    f = lambda a: np.ascontiguousarray(np.asarray(a, dtype=np.float32))
    x = f(x); c = f(c)
    w_in0 = f(w_in[0])
    perm = np.arange(1536)
    hd = perm % 64
    perm = perm - hd + (hd + 32) % 64
    w_perm = np.ascontiguousarray(w_in0[:, 1536:3072][:, perm])
    keysT = np.ascontiguousarray(
        np.transpose(f(peer_keys[0]).reshape(16, 128, 128), (2, 0, 1)))
    ident, tri_incl, tri_low, diagmask, dilmask = _consts()
    shared = {
        "w_ada": f(w_ada[0]), "b_ada": f(b_ada[0]).reshape(1, -1),
        "g_mix": f(g_mix[0]).reshape(1, -1), "g_ffn": f(g_ffn[0]).reshape(1, -1),
        "g_final": f(g_final).reshape(1, -1),
        "w_in": w_in0, "w_perm": w_perm, "w_sb_o": f(w_sb_o[0]), "w_dil_o": f(w_dil_o[0]),
        "w_out": f(w_out[0]), "w_pq": f(w_pq[0]), "keysT": keysT,
        "peer_u": f(peer_u[0]), "peer_v": f(peer_v[0]),
        "ident": ident, "tri_incl": tri_incl, "tri_low": tri_low, "diagmask": diagmask,
        "dilmask": dilmask,
    }
    ropes = {hh: _rope_tables(128 * (1 - hh)) for hh in (0, 1)}
    in_maps = []
    for core in range(8):
        b, hh = core // 2, core % 2
        if hh == 1:
            xall = x[b]
        else:
            xall = np.concatenate([np.zeros((128, D), np.float32), x[b, :SEQ - 128]], axis=0)
        m = dict(shared)
        m["xall"] = np.ascontiguousarray(xall)
        m["cT"] = np.ascontiguousarray(c[b].reshape(8, 128).T)
        m["kvalid"] = np.full((128, 1), float(hh), np.float32)
        m["ropeC"], m["ropeS"] = ropes[hh]
        in_maps.append(m)
    return in_maps


def kernel(**inputs):
    in_maps = make_in_maps(**inputs)
    nc = build_nc()
    res = run_bass_kernel_spmd(nc, in_maps, core_ids=list(range(8)))
    out = np.zeros((4, SEQ, D), np.float32)
    for core in range(8):
        b, hh = core // 2, core % 2
        y = np.asarray(res.results[core]["y"]).reshape(NOWN, 128, D)
        out[b].reshape(NB, 128, D)[hh::2] = y
    return out
```

```python
import contextlib
import numpy as np
import concourse.bass as bass
import concourse.mybir as mybir
from concourse.bass_utils import run_bass_kernel_spmd

F32 = mybir.dt.float32
BF16 = mybir.dt.bfloat16
AF = mybir.ActivationFunctionType
ALU = mybir.AluOpType
AX = mybir.AxisListType

D = 1024
SEQ = 8192
NB = 64
NOWN = 32
EPS = 1e-6
ENGS = ("pe", "act", "dve", "pool", "sp")
DIL = ((1, 2), (4, 5), (16, 17))
SAME_ENGINE_SYNC = True
DBG_SKIP = set()


class _Ins:
    __slots__ = ("eng", "fn", "waits", "signal", "sig_count", "is_dma", "dsem", "dcount")

    def __init__(self, eng, fn, is_dma=False):
        self.eng = eng
        self.fn = fn
        self.waits = []
        self.signal = False
        self.sig_count = 0
        self.is_dma = is_dma
        self.dsem = None
        self.dcount = 0


class Sched:
    def __init__(self, nc):
        self.nc = nc
        self.q = {e: [] for e in ENGS}
        self.last_w = {}
        self.readers = {}
        self.dma_counts = {}

    @staticmethod
    def _key(r):
        return r if isinstance(r, (tuple, str, int)) else id(r)

    def _dep(self, ins, other):
        if other is None or other is ins:
            return
        if other.is_dma:
            ins.waits.append(("D", other.dsem, other.dcount))
        elif other.eng != ins.eng or (SAME_ENGINE_SYNC and ins.eng != "pe"):
            other.signal = True
            ins.waits.append(("E", other.eng, other))

    def _track(self, ins, reads, writes):
        for r in reads:
            self._dep(ins, self.last_w.get(self._key(r)))
        for w in writes:
            k = self._key(w)
            self._dep(ins, self.last_w.get(k))
            rd = self.readers.get(k)
            if rd:
                for o in rd[0].values():
                    self._dep(ins, o)
                for o in rd[1]:
                    self._dep(ins, o)
        for r in reads:
            rd = self.readers.setdefault(self._key(r), ({}, []))
            if ins.is_dma:
                rd[1].append(ins)
            else:
                rd[0][ins.eng] = ins
        for w in writes:
            k = self._key(w)
            self.last_w[k] = ins
            self.readers[k] = ({}, [])

    def op(self, eng, fn, reads=(), writes=()):
        ins = _Ins(eng, fn)
        self._track(ins, reads, writes)
        self.q[eng].append(ins)
        return ins

    def dma(self, eng, fn, reads=(), writes=(), sem=None):
        ins = _Ins(eng, fn, is_dma=True)
        k = self._key(sem)
        self.dma_counts[k] = self.dma_counts.get(k, 0) + 16
        ins.dsem = k
        ins.dcount = self.dma_counts[k]
        self._track(ins, reads, writes)
        self.q[eng].append(ins)
        return ins

    def barrier(self):
        dsnap = dict(self.dma_counts)
        lastc = {}
        for f in ENGS:
            j = len(self.q[f]) - 1
            while j >= 0 and (self.q[f][j].is_dma or self.q[f][j].fn is None):
                j -= 1
            lastc[f] = self.q[f][j] if j >= 0 else None
        for e in ENGS:
            ins = _Ins(e, None)
            for f in ENGS:
                if f != e and lastc[f] is not None:
                    lastc[f].signal = True
                    ins.waits.append(("E", f, lastc[f]))
            for k, c in dsnap.items():
                ins.waits.append(("D", k, c))
            self.q[e].append(ins)
        self.last_w = {}
        self.readers = {}

    def emit(self):
        nc = self.nc
        for e in ENGS:
            c = 0
            for ins in self.q[e]:
                if ins.signal:
                    c += 1
                    ins.sig_count = c
        dkeys = list(self.dma_counts.keys())
        with contextlib.ExitStack() as es:
            esem = {e: es.enter_context(nc.semaphore("s_" + e)) for e in ENGS}
            dsem = {k: es.enter_context(nc.semaphore("d%d" % i)) for i, k in enumerate(dkeys)}
            block = es.enter_context(nc.Block())
            final_d = dict(self.dma_counts)

            def make(e):
                def body(engobj):
                    known_e = {f: 0 for f in ENGS}
                    known_d = {}
                    for ins in self.q[e]:
                        for w in ins.waits:
                            if w[0] == "E":
                                f, c = w[1], w[2].sig_count
                                if known_e[f] >= c:
                                    continue
                                known_e[f] = c
                                engobj.wait_ge(esem[f], c)
                            else:
                                k, c = w[1], w[2]
                                if known_d.get(k, 0) >= c:
                                    continue
                                known_d[k] = c
                                engobj.wait_ge(dsem[k], c)
                        if ins.fn is None:
                            continue
                        r = ins.fn(engobj)
                        if ins.is_dma:
                            r.then_inc(dsem[ins.dsem], 16)
                        elif ins.signal:
                            r.then_inc(esem[e], 1)
                    if e == "sp":
                        for k, c in final_d.items():
                            if known_d.get(k, 0) < c:
                                engobj.wait_ge(dsem[k], c)
                return body

            block.tensor(make("pe"))
            block.scalar(make("act"))
            block.vector(make("dve"))
            block.gpsimd(make("pool"))
            block.sync(make("sp"))


class Rot:
    def __init__(self, tiles):
        self.tiles = tiles
        self.i = 0

    def next(self):
        t = self.tiles[self.i % len(self.tiles)]
        self.i += 1
        return t


def build_nc(debug=None, NPAIR_SB=4, NPAIR_DIL=2, NEXP_BLK=128, NTILE_PEER=8, NTT=16, NQT=8, NOWN_EFF=32):
    nc = bass.Bass("TRN2", target_bir_lowering=False)
    S = Sched(nc)

    def din(name, shape, dt=F32):
        return nc.dram_tensor(name, list(shape), dt, kind="ExternalInput").ap()

    xall = din("xall", [SEQ, D])
    cT = din("cT", [128, 8])
    kvalid_d = din("kvalid", [128, 1])
    ropeC_d = din("ropeC", [128, SEQ])
    ropeS_d = din("ropeS", [128, SEQ])
    w_ada = din("w_ada", [D, 6 * D])
    b_ada = din("b_ada", [1, 6 * D])
    gmix_d = din("g_mix", [1, D])
    gffn_d = din("g_ffn", [1, D])
    gfin_d = din("g_final", [1, D])
    w_in = din("w_in", [D, 5888])
    w_perm = din("w_perm", [D, 1536])
    w_sb_o = din("w_sb_o", [512, D])
    w_dil_o = din("w_dil_o", [256, D])
    w_out = din("w_out", [D, D])
    w_pq = din("w_pq", [D, 2048])
    keysT_d = din("keysT", [128, 16, 128])
    peer_u = din("peer_u", [16384, D])
    peer_v = din("peer_v", [16384, D])
    ident_d = din("ident", [128, 128])
    triI_d = din("tri_incl", [128, 128])
    triL_d = din("tri_low", [128, 128])
    dmask_d = din("diagmask", [128, 128])
    dilm_d = din("dilmask", [128, 24, 128])

    y_out = nc.dram_tensor("y", [NOWN * 128, D], F32, kind="ExternalOutput").ap()
    dbg_out = None
    if debug:
        dbg_out = nc.dram_tensor("dbg", [128, 32768], F32, kind="ExternalOutput").ap()

    def dscr(name, shape, dt):
        return nc.dram_tensor(name, list(shape), dt, kind="Internal").ap()

    hT_d = dscr("hT_scr", [16, 128, 8, 512], BF16)
    x1_d = dscr("x1_scr", [NOWN, 128, D], F32)
    uT_d = dscr("uT_scr", [128, 128, 8, 128], BF16)
    vB_d = dscr("vB_scr", [128, 128, D], BF16)
    rt_f_d = dscr("rt_f", [NOWN, 128, 2, 8, 128], F32)
    kap_d = dscr("kap", [NOWN, 128, 8], F32)
    h2T_d = dscr("h2T_scr", [NOWN, 128, 8, 128], BF16)

    w_in_r = w_in.rearrange("(k p) n -> p k n", p=128)
    w_perm_r = w_perm.rearrange("(k p) n -> p k n", p=128)

    top = contextlib.ExitStack()

    def T(es, name, shape, dt=F32):
        return es.enter_context(nc.sbuf_tensor(name, list(shape), dt))

    def P(es, name, shape, dt=F32):
        return es.enter_context(nc.psum_tensor(name, list(shape), dt))

    def mm(out, lhsT, rhs, start, stop, reads, writes):
        S.op("pe", lambda e: e.matmul(out, lhsT=lhsT, rhs=rhs, start=start, stop=stop),
             reads, writes)

    def tr(out, in_, ident, reads, writes):
        S.op("pe", lambda e: e.transpose(out=out, in_=in_, identity=ident), reads, writes)

    def act(out, in_, func, reads, writes, bias=None, scale=None, accum=None):
        def f(e):
            kw = {}
            if bias is not None:
                kw["bias"] = bias
            if scale is not None:
                kw["scale"] = scale
            if accum is not None:
                kw["accum_out"] = accum
            return e.activation(out=out, in_=in_, func=func, **kw)
        S.op("act", f, reads, writes)

    def tt(eng, out, in0, in1, op, reads, writes):
        S.op(eng, lambda e: e.tensor_tensor(out=out, in0=in0, in1=in1, op=op), reads, writes)

    def ts(eng, out, in0, s1, s2, op0, op1, reads, writes):
        if s2 is None:
            S.op(eng, lambda e: e.tensor_scalar(out=out, in0=in0, scalar1=s1, scalar2=None,
                                                op0=op0), reads, writes)
        else:
            S.op(eng, lambda e: e.tensor_scalar(out=out, in0=in0, scalar1=s1, scalar2=s2,
                                                op0=op0, op1=op1), reads, writes)

    def stt(eng, out, in0, scalar, in1, op0, op1, reads, writes):
        S.op(eng, lambda e: e.scalar_tensor_tensor(out=out, in0=in0, scalar=scalar, in1=in1,
                                                   op0=op0, op1=op1), reads, writes)

    def cp(eng, out, in_, reads, writes):
        S.op(eng, lambda e: e.tensor_copy(out=out, in_=in_), reads, writes)

    def dma(eng, out, in_, reads, writes, sem):
        S.dma(eng, lambda e: e.dma_start(out=out, in_=in_), reads, writes, sem)

    identb = T(top, "identb", [128, 128], BF16)
    identf = T(top, "identf", [128, 128], F32)
    onesb = T(top, "onesb", [128, 128], BF16)
    onesf = T(top, "onesf", [128, 128], F32)
    kval = T(top, "kval", [128, 1], F32)
    epsT = T(top, "epsT", [128, 1], F32)
    G2b = T(top, "G2b", [128, D])
    SH2b = T(top, "SH2b", [128, D])
    GT1b = T(top, "GT1b", [128, D])
    GT2b = T(top, "GT2b", [128, D])
    GFb = T(top, "GFb", [128, D])
    es_attn = contextlib.ExitStack()
    osbT = T(es_attn, "osbT", [128, 4, NOWN * 128], BF16)
    odT = T(es_attn, "odT", [128, 2, NOWN * 128], BF16)
    es01 = contextlib.ExitStack()
    G1b = T(es01, "G1b", [128, D])
    SH1b = T(es01, "SH1b", [128, D])

    with contextlib.ExitStack() as es:
        scT = T(es, "scT", [128, 8])
        scB = T(es, "scB", [128, 8, 128])
        wad = T(es, "wad", [128, 8, 1536])
        modb = T(es, "modb", [128, 6 * D])
        badab = T(es, "badab", [128, 6 * D])
        gtmp = T(es, "gtmp", [128, D])
        ps0 = [P(es, "ps0_%d" % i, [128, 512]) for i in range(3)]

        dma("sp", identf[:, :], ident_d, [], [identf], identf)
        dma("sp", kval[:, :], kvalid_d, [], [kval], kval)
        dma("sp", scT[:, :], cT, [], [scT], scT)
        dma("sp", badab[:, :], b_ada.partition_broadcast(128)[:, 0, :], [], [badab], badab)
        dma("sp", GFb[:, :], gfin_d.partition_broadcast(128)[:, 0, :], [], [GFb], GFb)
        cp("dve", identb[:, :], identf[:, :], [identf], [identb])
        S.op("dve", lambda e: e.memset(onesf[:, :], 1.0), [], [onesf])
        S.op("dve", lambda e: e.memset(onesb[:, :], 1.0), [], [onesb])
        S.op("dve", lambda e: e.memset(epsT[:, :], EPS), [], [epsT])
        act(scT[:, :], scT[:, :], AF.Silu, [scT], [scT])
        for kc in range(8):
            ts("dve", scB[:, kc, :], onesf[:, :], scT[:, kc:kc + 1], None, ALU.mult, None,
               [onesf, scT], [scB])
        w_ada_r = w_ada.rearrange("(k p) n -> p k n", p=128)
        for g in range(4):
            dma("sp", wad[:, :, :], w_ada_r[:, :, g * 1536:(g + 1) * 1536], [], [wad], wad)
            for j in range(3):
                for kc in range(8):
                    mm(ps0[j][:, :], scB[:, kc, :], wad[:, kc, j * 512:(j + 1) * 512],
                       kc == 0, kc == 7, [scB, wad], [ps0[j]])
                c0 = g * 1536 + j * 512
                tt("dve", modb[:, c0:c0 + 512], ps0[j][:, :], badab[:, c0:c0 + 512], ALU.add,
                   [ps0[j], badab], [modb])
        cp("dve", SH1b[:, :], modb[:, 0:D], [modb], [SH1b])
        cp("dve", GT1b[:, :], modb[:, 2 * D:3 * D], [modb], [GT1b])
        cp("dve", SH2b[:, :], modb[:, 3 * D:4 * D], [modb], [SH2b])
        cp("dve", GT2b[:, :], modb[:, 5 * D:6 * D], [modb], [GT2b])
        dma("sp", gtmp[:, :], gmix_d.partition_broadcast(128)[:, 0, :], [], [gtmp], gtmp)
        stt("dve", G1b[:, :], modb[:, D:2 * D], 1.0, gtmp[:, :], ALU.add, ALU.mult,
            [modb, gtmp], [G1b])
        dma("sp", gtmp[:, :], gffn_d.partition_broadcast(128)[:, 0, :], [G1b], [gtmp], gtmp)
        stt("dve", G2b[:, :], modb[:, 4 * D:5 * D], 1.0, gtmp[:, :], ALU.add, ALU.mult,
            [modb, gtmp], [G2b])
        S.barrier()

    def norm_mod_T(x_t, Gb, SHb, junk, ss, rstd, xn, hblk, pT, outT, res):
        act(junk[:, :], x_t[:, :], AF.Square, [x_t], [junk, ss], accum=ss[:, :])
        act(rstd[:, :], ss[:, :], AF.Sqrt, [ss, epsT], [rstd], bias=epsT[:, :], scale=1.0 / D)
        S.op("dve", lambda e: e.reciprocal(out=rstd[:, :], in_=rstd[:, :]), [rstd], [rstd])
        stt("dve", xn[:, :], x_t[:, :], rstd[:, 0:1], Gb[:, :], ALU.mult, ALU.mult,
            [x_t, rstd, Gb], [xn])
        tt("dve", hblk[:, :], xn[:, :], SHb[:, :], ALU.add, [xn, SHb], [hblk])
        for kc in range(8):
            tr(pT[:, kc, :], hblk[:, kc * 128:(kc + 1) * 128], identb[:, :],
               [hblk, identb], [pT])
        act(outT, pT[:, :, :], AF.Copy, [pT], [res])

    with contextlib.ExitStack() as es:
        xr = Rot([T(es, "p1x%d" % i, [128, D]) for i in range(3)])
        junk = T(es, "p1junk", [128, D], BF16)
        ssr = Rot([T(es, "p1ss%d" % i, [128, 1]) for i in range(2)])
        rsr = Rot([T(es, "p1rs%d" % i, [128, 1]) for i in range(2)])
        xnr = Rot([T(es, "p1xn%d" % i, [128, D]) for i in range(2)])
        hbr = Rot([T(es, "p1hb%d" % i, [128, D], BF16) for i in range(2)])
        hTr = Rot([T(es, "p1hT%d" % i, [128, 8, 512], BF16) for i in range(2)])
        pTr = Rot([P(es, "p1pT%d" % i, [128, 8, 128], BF16) for i in range(2)])
        for tt_i in range(NTT):
            hTt = hTr.next()
            for j in range(4):
                tb = tt_i * 4 + j
                x_t = xr.next()
                dma("sp", x_t[:, :], xall[tb * 128:(tb + 1) * 128, :], [], [x_t], x_t)
                norm_mod_T(x_t, G1b, SH1b, junk, ssr.next(), rsr.next(), xnr.next(), hbr.next(),
                           pTr.next(), hTt[:, :, j * 128:(j + 1) * 128], hTt)
            dma("pool", hT_d[tt_i], hTt[:, :, :], [hTt], [("hT", tt_i)], hTt)
        S.barrier()
    es01.close()

    def load_w_bf16(dst, src_ap, n, stage_rot, eng="dve", c_off=0):
        step = 128
        for c0 in range(0, n, step):
            st = stage_rot.next()
            dma("sp", st[:, :, :], src_ap[:, :, c0:c0 + step], [], [st], st)
            cp(eng, dst[:, :, c_off + c0:c_off + c0 + step], st[:, :, :], [st], [dst])

    with contextlib.ExitStack() as es:
        stg = Rot([T(es, "p2stg%d" % i, [128, 8, 128]) for i in range(2)])
        Wq = T(es, "p2Wq", [128, 8, 128], BF16)
        Wk = T(es, "p2Wk", [128, 8, 128], BF16)
        Wv = T(es, "p2Wv", [128, 8, 128], BF16)
        KT = T(es, "p2KT", [128, SEQ], BF16)
        V = T(es, "p2V", [128, NB, 128], BF16)
        QT = T(es, "p2QT", [128, NOWN * 128], BF16)
        hTr = Rot([T(es, "p2hT%d" % i, [128, 8, 512], BF16) for i in range(2)])
        triI = T(es, "p2triI", [128, 128], BF16)
        triL = T(es, "p2triL", [128, 128], BF16)
        dmk = T(es, "p2dmk", [128, 128], F32)
        tmpf = T(es, "p2tmpf", [128, 128], F32)
        Et = [Rot([T(es, "p2E%d_%d" % (h, i), [128, 512]) for i in range(2)]) for h in range(2)]
        Lt = [Rot([T(es, "p2L%d_%d" % (h, i), [128, 512], BF16) for i in range(2)]) for h in range(2)]
        EXt = [Rot([T(es, "p2X%d_%d" % (h, i), [128, 512]) for i in range(2)]) for h in range(2)]
        At = [Rot([T(es, "p2A%d_%d" % (h, i), [128, 512], BF16) for i in range(2)]) for h in range(2)]
        Zb4 = [P(es, "p2Z%d" % i, [128, 512]) for i in range(4)]
        Zp = [Rot(Zb4[0:2]), Rot(Zb4[2:4])]
        Cp = [P(es, "p2C%d" % h, [128, 512]) for h in range(2)]
        Op = [P(es, "p2O%d" % h, [128, 512]) for h in range(2)]
        Gp = Rot(Zb4)

        dma("sp", tmpf[:, :], triI_d, [], [tmpf], tmpf)
        cp("dve", triI[:, :], tmpf[:, :], [tmpf], [triI])
        dma("sp", tmpf[:, :], triL_d, [triI], [tmpf], tmpf)
        cp("dve", triL[:, :], tmpf[:, :], [tmpf], [triL])
        dma("sp", dmk[:, :], dmask_d, [], [dmk], dmk)

        if NPAIR_SB == 0:
            for pr in range(4):
                S.op("pool", lambda e, pr=pr: e.memset(osbT[:, pr, :], 0.0), [],
                     [("osbT", pr, q) for q in range(8)])
        for pair in range(NPAIR_SB):
            load_w_bf16(Wq, w_in_r[:, :, pair * 128:(pair + 1) * 128], 128, stg)
            load_w_bf16(Wk, w_in_r[:, :, 512 + pair * 128:512 + (pair + 1) * 128], 128, stg)
            load_w_bf16(Wv, w_in_r[:, :, 1024 + pair * 128:1024 + (pair + 1) * 128], 128, stg)
            for tt_i in range(NTT):
                hTt = hTr.next()
                dma("sp", hTt[:, :, :], hT_d[tt_i], [("hT", tt_i)], [hTt], hTt)
                g = Gp.next()
                for kc in range(8):
                    mm(g[:, :], Wk[:, kc, :], hTt[:, kc, :], kc == 0, kc == 7, [Wk, hTt], [g])
                cp("dve", KT[:, tt_i * 512:(tt_i + 1) * 512], g[:, :], [g], [("KT", tt_i)])
                g = Gp.next()
                for j in range(4):
                    for kc in range(8):
                        mm(g[:, j * 128:(j + 1) * 128], hTt[:, kc, j * 128:(j + 1) * 128],
                           Wv[:, kc, :], kc == 0, kc == 7, [Wv, hTt], [g])
                act(V[:, tt_i * 4:(tt_i + 1) * 4, :], g[:, :].rearrange("p (j c) -> p j c", j=4),
                    AF.Copy, [g], [("V", tt_i)])
                g = Gp.next()
                for jj, j in enumerate((1, 3)):
                    for kc in range(8):
                        mm(g[:, jj * 128:(jj + 1) * 128], Wq[:, kc, :],
                           hTt[:, kc, j * 128:(j + 1) * 128], kc == 0, kc == 7, [Wq, hTt], [g])
                n0 = tt_i * 2
                act(QT[:, n0 * 128:(n0 + 2) * 128], g[:, 0:256], AF.Copy, [g], [("QT", n0 // 4)],
                    scale=0.125)
            for qi in range(NQT):
                kb_top = 8 * qi + 7
                qres = ("QT", qi)
                st = {}

                def stage1_pe(kb, h):
                    jmin = max(0, (kb - 8 * qi) // 2)
                    c0 = jmin * 128
                    r0, r1 = h * 64, (h + 1) * 64
                    Z = Zp[h].next()
                    st[(kb, h)] = dict(c0=c0, Z=Z, diag=(kb == 8 * qi + 2 * jmin + 1))
                    mm(Z[:, c0:512], KT[r0:r1, kb * 128:(kb + 1) * 128],
                       QT[r0:r1, qi * 512 + c0:(qi + 1) * 512], True, True,
                       [("KT", kb // 4), qres], [Z])

                def stage1_el(kb, h):
                    d = st[(kb, h)]
                    c0, Z = d["c0"], d["Z"]
                    E = Et[h].next(); L = Lt[h].next()
                    d["E"], d["L"] = E, L
                    act(E[:, c0:512], Z[:, c0:512], AF.Exp, [Z], [E])
                    if d["diag"]:
                        tt("dve", E[:, c0:c0 + 128], E[:, c0:c0 + 128], dmk[:, :], ALU.mult,
                           [E, dmk], [E])
                    if kb == 0:
                        ts("dve", E[:, c0:512], E[:, c0:512], kval[:, 0:1], None, ALU.mult,
                           None, [E, kval], [E])
                    act(L[:, c0:512], E[:, c0:512], AF.Ln, [E], [L], bias=1.0)

                def c1(kb, h):
                    d = st[(kb, h)]
                    c0 = d["c0"]
                    mm(Cp[h][:, c0:512], triI[:, :], d["L"][:, c0:512], kb == kb_top, True,
                       [triI, d["L"]], [Cp[h]])

                def exa(kb, h):
                    d = st[(kb, h)]
                    c0 = d["c0"]
                    EX = EXt[h].next(); A = At[h].next()
                    d["A"] = A
                    act(EX[:, c0:512], Cp[h][:, c0:512], AF.Exp, [Cp[h]], [EX], scale=-1.0)
                    tt("dve", A[:, c0:512], d["E"][:, c0:512], EX[:, c0:512], ALU.mult,
                       [d["E"], EX], [A])

                def oc2(kb, h):
                    d = st[(kb, h)]
                    c0 = d["c0"]
                    mm(Op[h][:, c0:512], V[:, kb, :], d["A"][:, c0:512], kb == kb_top, kb == 0,
                       [("V", kb // 4), d["A"]], [Op[h]])
                    if kb > 0:
                        mm(Cp[h][:, c0:512], triL[:, :], d["L"][:, c0:512], False, False,
                           [triL, d["L"]], [Cp[h]])

                for h in range(2):
                    stage1_pe(kb_top, h)
                for h in range(2):
                    stage1_el(kb_top, h)
                for kb in range(kb_top, -1, -1):
                    for h in range(2):
                        c1(kb, h)
                    if kb > 0:
                        for h in range(2):
                            stage1_pe(kb - 1, h)
                    for h in range(2):
                        exa(kb, h)
                    if kb > 0:
                        for h in range(2):
                            stage1_el(kb - 1, h)
                    for h in range(2):
                        oc2(kb, h)
                for h in range(2):
                    r0, r1 = h * 64, (h + 1) * 64
                    act(osbT[r0:r1, pair, qi * 512:(qi + 1) * 512], Op[h][r0:r1, :], AF.Copy,
                        [Op[h]], [("osbT", pair, qi)])
        S.barrier()

    if debug == "sb":
        with contextlib.ExitStack() as es:
            st = Rot([T(es, "dbgst%d" % i, [128, 2048]) for i in range(2)])
            for pr in range(4):
                for c in range(2):
                    s_ = st.next()
                    cp("dve", s_[:, :], osbT[:, pr, c * 2048:(c + 1) * 2048],
                       [("osbT", pr, q) for q in range(8)], [s_])
                    dma("sp", dbg_out[:, pr * 4096 + c * 2048: pr * 4096 + (c + 1) * 2048], s_[:, :],
                        [s_], [], s_)
        es_attn.close(); top.close()
        S.emit()
        return nc

    with contextlib.ExitStack() as es:
        stg = Rot([T(es, "p3stg%d" % i, [128, 8, 128]) for i in range(2)])
        Wq = T(es, "p3Wq", [128, 8, 128], BF16)
        Wk = T(es, "p3Wk", [128, 8, 128], BF16)
        Wv = T(es, "p3Wv", [128, 8, 128], BF16)
        Wqp = T(es, "p3Wqp", [128, 8, 128], BF16)
        Wkp = T(es, "p3Wkp", [128, 8, 128], BF16)
        KT = T(es, "p3KT", [128, SEQ], BF16)
        V = T(es, "p3V", [128, NB, 128], BF16)
        QT = T(es, "p3QT", [128, NOWN * 256], BF16)
        hTr = Rot([T(es, "p3hT%d" % i, [128, 8, 512], BF16) for i in range(2)])
        Cr = Rot([T(es, "p3C%d" % i, [128, 512]) for i in range(2)])
        Sr = Rot([T(es, "p3S%d" % i, [128, 512]) for i in range(2)])
        t1r = Rot([T(es, "p3t1%d" % i, [128, 512]) for i in range(2)])
        t2r = Rot([T(es, "p3t2%d" % i, [128, 512]) for i in range(2)])
        dilm = T(es, "p3dilm", [128, 24, 128], BF16)
        Er = Rot([T(es, "p3E%d" % i, [128, 256], BF16) for i in range(3)])
        Oacc = T(es, "p3Oacc", [128, NOWN * 128], F32)
        Zacc = T(es, "p3Zacc", [128, NOWN * 128], F32)
        Gp = Rot([P(es, "p3G%d" % i, [128, 512]) for i in range(4)])
        Zr = Rot([P(es, "p3Z%d" % i, [128, 512]) for i in range(2)])
        Ob = P(es, "p3Ob", [128, 512])
        Zs = P(es, "p3Zs", [128, 512])

        for m0 in range(0, 24, 8):
            st = stg.next()
            dma("sp", st[:, :, :], dilm_d[:, m0:m0 + 8, :], [], [st], st)
            cp("dve", dilm[:, m0:m0 + 8, :], st[:, :, :], [st], [dilm])
        mbase = (0, 2, 7)
        S.op("pool", lambda e: e.memset(QT[:, :], 0.0), [], [("QT", q) for q in range(8)])
        PENG = "dve" if "p" in DBG_SKIP else "pool"
        for sp_i in range(NPAIR_DIL):
            for g, (r, nkb) in enumerate(DIL):
                hc = (g * 4 + 2 * sp_i) * 64
                load_w_bf16(Wq, w_in_r[:, :, 1536 + hc:1536 + hc + 128], 128, stg)
                load_w_bf16(Wk, w_in_r[:, :, 2304 + hc:2304 + hc + 128], 128, stg)
                load_w_bf16(Wv, w_in_r[:, :, 3072 + hc:3072 + hc + 128], 128, stg)
                load_w_bf16(Wqp, w_perm_r[:, :, hc:hc + 128], 128, stg)
                load_w_bf16(Wkp, w_perm_r[:, :, 768 + hc:768 + hc + 128], 128, stg)
                for tt_i in range(NTT):
                    hTt = hTr.next(); Ct = Cr.next(); St = Sr.next()
                    dma("sp", hTt[:, :, :], hT_d[tt_i], [("hT", tt_i)], [hTt], hTt)
                    dma("sp", Ct[:, :], ropeC_d[:, tt_i * 512:(tt_i + 1) * 512], [], [Ct], Ct)
                    dma("sp", St[:, :], ropeS_d[:, tt_i * 512:(tt_i + 1) * 512], [], [St], St)
                    g1 = Gp.next(); g2 = Gp.next(); t1 = t1r.next(); t2 = t2r.next()
                    for kc in range(8):
                        mm(g1[:, :], Wk[:, kc, :], hTt[:, kc, :], kc == 0, kc == 7, [Wk, hTt], [g1])
                    for kc in range(8):
                        mm(g2[:, :], Wkp[:, kc, :], hTt[:, kc, :], kc == 0, kc == 7, [Wkp, hTt], [g2])
                    tt("dve", t1[:, :], g1[:, :], Ct[:, :], ALU.mult, [g1, Ct], [t1])
                    tt("dve", t2[:, :], g2[:, :], St[:, :], ALU.mult, [g2, St], [t2])
                    tt(PENG, KT[:, tt_i * 512:(tt_i + 1) * 512], t1[:, :], t2[:, :], ALU.add,
                       [t1, t2], [("KT", tt_i)])
                    g3 = Gp.next()
                    for j in range(4):
                        for kc in range(8):
                            mm(g3[:, j * 128:(j + 1) * 128], hTt[:, kc, j * 128:(j + 1) * 128],
                               Wv[:, kc, :], kc == 0, kc == 7, [Wv, hTt], [g3])
                    act(V[:, tt_i * 4:(tt_i + 1) * 4, :], g3[:, :].rearrange("p (j c) -> p j c", j=4),
                        AF.Copy, [g3], [("V", tt_i)])
                    g1 = Gp.next(); g2 = Gp.next(); t1 = t1r.next(); t2 = t2r.next()
                    for jj, j in enumerate((1, 3)):
                        for kc in range(8):
                            mm(g1[:, jj * 128:(jj + 1) * 128], Wq[:, kc, :],
                               hTt[:, kc, j * 128:(j + 1) * 128], kc == 0, kc == 7, [Wq, hTt], [g1])
                        for kc in range(8):
                            mm(g2[:, jj * 128:(jj + 1) * 128], Wqp[:, kc, :],
                               hTt[:, kc, j * 128:(j + 1) * 128], kc == 0, kc == 7, [Wqp, hTt], [g2])
                    Cv = Ct[:, :].rearrange("p (j c) -> p j c", j=4)[:, 1::2, :]
                    Sv = St[:, :].rearrange("p (j c) -> p j c", j=4)[:, 1::2, :]
                    v3 = lambda ap: ap.rearrange("p (j c) -> p j c", j=2)
                    tt("dve", v3(t1[:, 0:256]), v3(g1[:, 0:256]), Cv, ALU.mult, [g1, Ct], [t1])
                    tt("dve", v3(t2[:, 0:256]), v3(g2[:, 0:256]), Sv, ALU.mult, [g2, St], [t2])
                    n0 = tt_i * 2
                    for h in range(2):
                        r0, r1 = h * 64, (h + 1) * 64
                        qv = QT[r0:r1, n0 * 256:(n0 + 2) * 256].rearrange(
                            "p (j c) -> p j c", j=2)[:, :, h * 128:(h + 1) * 128]
                        tt(PENG, qv, v3(t1[r0:r1, 0:256]), v3(t2[r0:r1, 0:256]), ALU.add,
                           [t1, t2], [("QT", n0 // 4)])
                flat = []
                for n in range(0 if "a" in DBG_SKIP else NOWN_EFF):
                    Pb = 2 * n + 1
                    steps = [dl for dl in range(nkb) if Pb - dl >= 0]
                    for si, dl in enumerate(steps):
                        flat.append((n, si, dl, len(steps)))
                stA = {}

                def stageA(k):
                    n, si, dl, ns = flat[k]
                    kb = 2 * n + 1 - dl
                    Zb = Zr.next(); E = Er.next()
                    mm(Zb[:, 0:256], KT[:, kb * 128:(kb + 1) * 128],
                       QT[:, n * 256:(n + 1) * 256], True, True,
                       [("KT", kb // 4), ("QT", n // 4)], [Zb])
                    act(E[:, :], Zb[:, 0:256], AF.Exp, [Zb], [E], scale=0.125)
                    mi = mbase[g] + dl
                    tt(PENG, E[:, :].rearrange("p (h q) -> p h q", h=2),
                       E[:, :].rearrange("p (h q) -> p h q", h=2),
                       dilm[:, mi:mi + 1, :].to_broadcast([128, 2, 128]), ALU.mult,
                       [E, dilm], [E])
                    if kb == 0:
                        ts("dve", E[:, :], E[:, :], kval[:, 0:1], None, ALU.mult, None,
                           [E, kval], [E])
                    stA[k] = E

                def stageB(k):
                    n, si, dl, ns = flat[k]
                    kb = 2 * n + 1 - dl
                    E = stA.pop(k)
                    mm(Ob[:, 0:256], V[:, kb, :], E[:, :], si == 0, si == ns - 1,
                       [("V", kb // 4), E], [Ob])
                    mm(Zs[:, 0:256], onesb[:, :], E[:, :], si == 0, si == ns - 1,
                       [onesb, E], [Zs])
                    if si == ns - 1:
                        for h in range(2):
                            r0, r1 = h * 64, (h + 1) * 64
                            oa = Oacc[r0:r1, n * 128:(n + 1) * 128]
                            za = Zacc[r0:r1, n * 128:(n + 1) * 128]
                            if g == 0:
                                cp("dve", oa, Ob[r0:r1, h * 128:(h + 1) * 128], [Ob], [Oacc])
                                cp("dve", za, Zs[r0:r1, h * 128:(h + 1) * 128], [Zs], [Zacc])
                            else:
                                tt("dve", oa, Ob[r0:r1, h * 128:(h + 1) * 128], oa, ALU.add,
                                   [Ob, Oacc], [Oacc])
                                tt("dve", za, Zs[r0:r1, h * 128:(h + 1) * 128], za, ALU.add,
                                   [Zs, Zacc], [Zacc])

                if flat:
                    stageA(0)
                for k in range(len(flat)):
                    if k + 1 < len(flat):
                        stageA(k + 1)
                    stageB(k)
            NE = NOWN_EFF * 128
            S.op("dve", lambda e: e.reciprocal(out=Zacc[:, 0:NE], in_=Zacc[:, 0:NE]), [Zacc], [Zacc])
            tt("dve", odT[:, sp_i, 0:NE], Oacc[:, 0:NE], Zacc[:, 0:NE], ALU.mult, [Oacc, Zacc],
               [("odT", sp_i)])
        S.barrier()

    if debug == "dil":
        with contextlib.ExitStack() as es:
            st = Rot([T(es, "dbgst%d" % i, [128, 2048]) for i in range(2)])
            for pr in range(2):
                for c in range(2):
                    s_ = st.next()
                    cp("dve", s_[:, :], odT[:, pr, c * 2048:(c + 1) * 2048], [("odT", pr)], [s_])
                    dma("sp", dbg_out[:, pr * 4096 + c * 2048: pr * 4096 + (c + 1) * 2048], s_[:, :],
                        [s_], [], s_)
        es_attn.close(); top.close()
        S.emit()
        return nc

    with contextlib.ExitStack() as es:
        stg = Rot([T(es, "p4stg%d" % i, [128, 8, 128]) for i in range(2)])
        Wg = T(es, "p4Wg", [128, 8, 2048], BF16)
        Wsbo = T(es, "p4Wsbo", [128, 4, 1024], BF16)
        Wdo = T(es, "p4Wdo", [128, 2, 1024], BF16)
        Wout = T(es, "p4Wout", [128, 8, 1024], BF16)
        hBr = Rot([T(es, "p4hB%d" % i, [128, 8, 128], BF16) for i in range(2)])
        sg = T(es, "p4sg", [128, 2048], BF16)
        m1 = T(es, "p4m1", [128, D])
        m2 = T(es, "p4m2", [128, D])
        mg = T(es, "p4mg", [128, D], BF16)
        mT = T(es, "p4mT", [128, 8, 128], BF16)
        xr = Rot([T(es, "p4x%d" % i, [128, D]) for i in range(2)])
        x1r = Rot([T(es, "p4x1%d" % i, [128, D]) for i in range(2)])
        tmpr = T(es, "p4tmp", [128, D])
        pg = [P(es, "p4pg%d" % i, [128, 512]) for i in range(4)]
        pbs = [P(es, "p4pbs%d" % i, [128, 512]) for i in range(2)]
        pbd = [P(es, "p4pbd%d" % i, [128, 512]) for i in range(2)]

        load_w_bf16(Wg, w_in_r[:, :, 3840:5888], 2048, stg)
        for t4 in range(4):
            for c0 in range(0, 1024, 512):
                st = stg.next()
                stv = st[:, :, :].rearrange("p a b -> p (a b)")
                dma("sp", stv[:, 0:512], w_sb_o[t4 * 128:(t4 + 1) * 128, c0:c0 + 512], [], [st], st)
                cp("dve", Wsbo[:, t4, c0:c0 + 512], stv[:, 0:512], [st], [Wsbo])
        for t2 in range(2):
            for c0 in range(0, 1024, 512):
                st = stg.next()
                stv = st[:, :, :].rearrange("p a b -> p (a b)")
                dma("sp", stv[:, 0:512], w_dil_o[t2 * 128:(t2 + 1) * 128, c0:c0 + 512], [], [st], st)
                cp("dve", Wdo[:, t2, c0:c0 + 512], stv[:, 0:512], [st], [Wdo])
        load_w_bf16(Wout, w_out.rearrange("(k p) n -> p k n", p=128), 1024, stg)

        for n in range(NOWN_EFF):
            tti, j = n // 2, 1 + 2 * (n % 2)
            hB = hBr.next()
            dma("sp", hB[:, :, :], hT_d[tti][:, :, j * 128:(j + 1) * 128], [("hT", tti)], [hB], hB)
            for c4 in range(4):
                for kc in range(8):
                    mm(pg[c4][:, :], hB[:, kc, :], Wg[:, kc, c4 * 512:(c4 + 1) * 512],
                       kc == 0, kc == 7, [hB, Wg], [pg[c4]])
                act(sg[:, c4 * 512:(c4 + 1) * 512], pg[c4][:, :], AF.Sigmoid, [pg[c4]], [sg])
            for nh in range(2):
                for t4 in range(4):
                    mm(pbs[nh][:, :], osbT[:, t4, n * 128:(n + 1) * 128],
                       Wsbo[:, t4, nh * 512:(nh + 1) * 512], t4 == 0, t4 == 3,
                       [("osbT", t4, n // 4), Wsbo], [pbs[nh]])
                for t2 in range(2):
                    mm(pbd[nh][:, :], odT[:, t2, n * 128:(n + 1) * 128],
                       Wdo[:, t2, nh * 512:(nh + 1) * 512], t2 == 0, t2 == 1,
                       [("odT", t2), Wdo], [pbd[nh]])
                tt("dve", m1[:, nh * 512:(nh + 1) * 512], pbs[nh][:, :],
                   sg[:, nh * 512:(nh + 1) * 512], ALU.mult, [pbs[nh], sg], [m1])
                tt("dve", m2[:, nh * 512:(nh + 1) * 512], pbd[nh][:, :],
                   sg[:, 1024 + nh * 512:1024 + (nh + 1) * 512], ALU.mult, [pbd[nh], sg], [m2])
            tt("pool", mg[:, :], m1[:, :], m2[:, :], ALU.add, [m1, m2], [mg])
            pTm = pg[0][:, :].bitcast(BF16).rearrange("p (k t) -> p k t", k=8)
            for kc in range(8):
                tr(pTm[:, kc, :], mg[:, kc * 128:(kc + 1) * 128], identb[:, :], [mg, identb], [pg[0]])
            act(mT[:, :, :], pTm, AF.Copy, [pg[0]], [mT])
            x_t = xr.next(); x1 = x1r.next()
            tb = 2 * n + 1
            dma("sp", x_t[:, :], xall[tb * 128:(tb + 1) * 128, :], [], [x_t], x_t)
            for nh in range(2):
                for kc in range(8):
                    mm(pg[1 + nh][:, :], mT[:, kc, :], Wout[:, kc, nh * 512:(nh + 1) * 512],
                       kc == 0, kc == 7, [mT, Wout], [pg[1 + nh]])
                tt("dve", tmpr[:, nh * 512:(nh + 1) * 512], pg[1 + nh][:, :],
                   GT1b[:, nh * 512:(nh + 1) * 512], ALU.mult, [pg[1 + nh], GT1b], [tmpr])
            tt("pool", x1[:, :], tmpr[:, :], x_t[:, :], ALU.add, [tmpr, x_t], [x1])
            dma("pool", x1_d[n], x1[:, :], [x1], [("x1", n)], x1)
            if debug == "x1":
                dma("pool", dbg_out[:, n * 1024:(n + 1) * 1024], x1[:, :], [x1], [], x1)
        S.barrier()
    es_attn.close()

    if debug == "x1":
        top.close()
        S.emit()
        return nc

    NEG = -1.0e30
    with contextlib.ExitStack() as es:
        stg = Rot([T(es, "p5stg%d" % i, [128, 8, 128]) for i in range(2)])
        Wpq = T(es, "p5Wpq", [128, 8, 2048], BF16)
        keysf = T(es, "p5keysf", [128, 16, 128], F32)
        keysb = T(es, "p5keysb", [128, 16, 128], BF16)
        xr = Rot([T(es, "p5x%d" % i, [128, D]) for i in range(2)])
        junk = T(es, "p5junk", [128, D], BF16)
        ss = T(es, "p5ss", [128, 1]); rstd = T(es, "p5rstd", [128, 1])
        xn = T(es, "p5xn", [128, D]); hblk = T(es, "p5hblk", [128, D], BF16)
        h2Br = Rot([T(es, "p5h2B%d" % i, [128, 8, 128], BF16) for i in range(2)])
        qT = T(es, "p5qT", [128, 16, 128], BF16)
        s_all = T(es, "p5sall", [128, 16, 128], F32)
        work = T(es, "p5work", [128, 16, 128], F32)
        top16 = T(es, "p5top16", [128, 16, 16], F32)
        cand = T(es, "p5cand", [128, 8, 256], F32)
        cw1 = T(es, "p5cw1", [128, 8, 256], F32)
        cw2 = T(es, "p5cw2", [128, 8, 256], F32)
        c24 = T(es, "p5c24", [128, 8, 24], F32)
        tau = T(es, "p5tau", [128, 8], F32)
        zt = T(es, "p5zt", [128, 8, 16], F32)
        Zsum = T(es, "p5Zsum", [128, 8], F32)
        d0 = T(es, "p5d0", [128, 8, 128], F32)
        e1 = T(es, "p5e1", [128, 8, 128], F32)
        rfr = Rot([T(es, "p5rf%d" % i, [128, 2, 8, 128], F32) for i in range(2)])
        tm8 = T(es, "p5tm8", [128, 8], F32)
        kpr = Rot([T(es, "p5kp%d" % i, [128, 8], F32) for i in range(2)])
        pT = P(es, "p5pT", [128, 8, 128], BF16)
        pq = [P(es, "p5pq%d" % i, [128, 512]) for i in range(4)]

        load_w_bf16(Wpq, w_pq.rearrange("(k p) n -> p k n", p=128), 2048, stg)
        dma("sp", keysf[:, :, :], keysT_d, [], [keysf], keysf)
        cp("dve", keysb[:, :, :], keysf[:, :, :], [keysf], [keysb])

        for n in range(NOWN_EFF):
            x_t = xr.next(); h2B = h2Br.next(); rf = rfr.next(); kp = kpr.next()
            dma("sp", x_t[:, :], x1_d[n], [("x1", n)], [x_t], x_t)
            norm_mod_T(x_t, G2b, SH2b, junk, ss, rstd, xn, hblk, pT, h2B[:, :, :], h2B)
            dma("pool", h2T_d[n], h2B[:, :, :], [h2B], [("h2T", n)], h2B)
            for hp in range(16):
                for kc in range(8):
                    mm(pq[hp // 4][:, (hp % 4) * 128:(hp % 4 + 1) * 128],
                       Wpq[:, kc, hp * 128:(hp + 1) * 128], h2B[:, kc, :], kc == 0, kc == 7,
                       [Wpq, h2B], [pq[hp // 4]])
            for b4 in range(4):
                act(qT[:, b4 * 4:(b4 + 1) * 4, :],
                    pq[b4][:, :].rearrange("p (a c) -> p a c", a=4), AF.Copy, [pq[b4]], [qT])
            for hp in range(16):
                mm(pq[hp // 4][:, (hp % 4) * 128:(hp % 4 + 1) * 128], qT[:, hp, :], keysb[:, hp, :],
                   True, True, [qT, keysb], [pq[hp // 4]])
            for b4 in range(4):
                cp("dve", s_all[:, b4 * 4:(b4 + 1) * 4, :],
                   pq[b4][:, :].rearrange("p (a c) -> p a c", a=4), [pq[b4]], [s_all])
            for hp in range(16):
                S.op("dve", lambda e, hp=hp: e.max(out=top16[:, hp, 0:8], in_=s_all[:, hp, :]),
                     [s_all], [top16])
                S.op("dve", lambda e, hp=hp: e.match_replace(
                    out=work[:, hp, :], in_to_replace=top16[:, hp, 0:8], in_values=s_all[:, hp, :],
                    imm_value=NEG), [s_all, top16], [work])
                S.op("dve", lambda e, hp=hp: e.max(out=top16[:, hp, 8:16], in_=work[:, hp, :]),
                     [work], [top16])
            t4v = top16[:, :, :].rearrange("p (h two) k -> p h two k", two=2)
            a0 = t4v[:, :, 0, :].unsqueeze(3).to_broadcast([128, 8, 16, 16])
            a1 = t4v[:, :, 1, :].unsqueeze(2).to_broadcast([128, 8, 16, 16])
            tt("dve", cand[:, :, :].rearrange("p h (a b) -> p h a b", a=16), a0, a1, ALU.add,
               [top16], [cand])
            for h in range(8):
                S.op("dve", lambda e, h=h: e.max(out=c24[:, h, 0:8], in_=cand[:, h, :]), [cand], [c24])
                S.op("dve", lambda e, h=h: e.match_replace(
                    out=cw1[:, h, :], in_to_replace=c24[:, h, 0:8], in_values=cand[:, h, :],
                    imm_value=NEG), [cand, c24], [cw1])
                S.op("dve", lambda e, h=h: e.max(out=c24[:, h, 8:16], in_=cw1[:, h, :]), [cw1], [c24])
                S.op("dve", lambda e, h=h: e.match_replace(
                    out=cw2[:, h, :], in_to_replace=c24[:, h, 8:16], in_values=cw1[:, h, :],
                    imm_value=NEG), [cw1, c24], [cw2])
                S.op("dve", lambda e, h=h: e.max(out=c24[:, h, 16:24], in_=cw2[:, h, :]), [cw2], [c24])
            tt("dve", tau[:, :], c24[:, :, 15], c24[:, :, 16], ALU.add, [c24], [tau])
            ts("dve", tau[:, :], tau[:, :], 0.5, None, ALU.mult, None, [tau], [tau])
            tt("dve", zt[:, :, :], c24[:, :, 0:16], c24[:, :, 0:1].to_broadcast([128, 8, 16]),
               ALU.subtract, [c24], [zt])
            act(zt[:, :, :], zt[:, :, :], AF.Exp, [zt], [zt])
            S.op("dve", lambda e: e.reduce_sum(out=Zsum[:, :], in_=zt[:, :, :], axis=AX.X),
                 [zt], [Zsum])
            S.op("dve", lambda e: e.reciprocal(out=Zsum[:, :], in_=Zsum[:, :]), [Zsum], [Zsum])
            s4 = s_all[:, :, :].rearrange("p (h two) j -> p h two j", two=2)
            s0v, s1v = s4[:, :, 0, :], s4[:, :, 1, :]
            m0v = t4v[:, :, 0, 0:1].to_broadcast([128, 8, 128])
            m1v = t4v[:, :, 1, 0:1].to_broadcast([128, 8, 128])
            tt("dve", d0[:, :, :], s1v, m1v, ALU.subtract, [s_all, top16], [d0])
            act(rf[:, 0, :, :], d0[:, :, :], AF.Exp, [d0], [rf])
            tt("dve", tm8[:, :], t4v[:, :, 1, 0], tau[:, :], ALU.subtract, [top16, tau], [tm8])
            tt("dve", e1[:, :, :], s0v, tm8[:, :].unsqueeze(2).to_broadcast([128, 8, 128]), ALU.add,
               [s_all, tm8], [e1])
            act(rf[:, 1, :, :], e1[:, :, :], AF.Exp, [e1], [rf])
            tt("dve", kp[:, :], tau[:, :], c24[:, :, 0], ALU.subtract, [tau, c24], [kp])
            act(kp[:, :], kp[:, :], AF.Exp, [kp], [kp])
            tt("dve", kp[:, :], kp[:, :], Zsum[:, :], ALU.mult, [kp, Zsum], [kp])
            dma("pool", rt_f_d[n], rf[:, :, :, :], [rf], [("rtf", n)], rf)
            dma("pool", kap_d[n], kp[:, :], [kp], [("kap", n)], kp)
        S.barrier()

    if debug == "rt":
        top.close()
        S.emit()
        return nc

    with contextlib.ExitStack() as es:
        ur = Rot([T(es, "p6u%d" % i, [128, D]) for i in range(2)])
        vr = Rot([T(es, "p6v%d" % i, [128, D]) for i in range(2)])
        ubr = Rot([T(es, "p6ub%d" % i, [128, D], BF16) for i in range(2)])
        vbr = Rot([T(es, "p6vb%d" % i, [128, D], BF16) for i in range(2)])
        uTr = Rot([T(es, "p6uT%d" % i, [128, 8, 128], BF16) for i in range(2)])
        pTr = Rot([P(es, "p6pT%d" % i, [128, 8, 128], BF16) for i in range(2)])
        for i in range(NEXP_BLK):
            u_t = ur.next(); v_t = vr.next(); ub = ubr.next(); vb = vbr.next(); uT = uTr.next()
            pT = pTr.next()
            dma("sp", u_t[:, :], peer_u[i * 128:(i + 1) * 128, :], [], [u_t], u_t)
            dma("sp", v_t[:, :], peer_v[i * 128:(i + 1) * 128, :], [], [v_t], v_t)
            act(ub[:, :], u_t[:, :], AF.Copy, [u_t], [ub])
            for kc in range(8):
                tr(pT[:, kc, :], ub[:, kc * 128:(kc + 1) * 128], identb[:, :], [ub, identb], [pT])
            cp("dve", uT[:, :, :], pT[:, :, :], [pT], [uT])
            dma("pool", uT_d[i], uT[:, :, :], [uT], [("uT", i)], uT)
            cp("dve", vb[:, :], v_t[:, :], [v_t], [vb])
            dma("pool", vB_d[i], vb[:, :], [vb], [("vB", i)], vb)
        S.barrier()

    if debug == "5a":
        top.close()
        S.emit()
        return nc

    IG = 4
    with contextlib.ExitStack() as es:
        h2t = T(es, "p7h2t", [128, 8, 512], BF16)
        rf = T(es, "p7rf", [128, 4, 2, 8, 128], F32)
        kp = T(es, "p7kp", [128, 4, 8], F32)
        dg = T(es, "p7dg", [128, 4, 8, 128], BF16)
        yacc = T(es, "p7yacc", [128, 4, D], F32)
        uTg_r = Rot([T(es, "p7uT%d" % i, [128, IG, 1024], BF16) for i in range(2)])
        vBg_r = Rot([T(es, "p7vB%d" % i, [128, IG, 1024], BF16) for i in range(2)])
        ga2_r = Rot([T(es, "p7ga2%d" % i, [128, IG, 512], BF16) for i in range(2)])
        eD_r = Rot([T(es, "p7eD%d" % i, [128, 4, 8, 128], F32) for i in range(2)])
        r1_r = Rot([T(es, "p7r1%d" % i, [128, 4, 8, 128], BF16) for i in range(2)])
        ga_r = Rot([T(es, "p7ga%d" % i, [128, 512]) for i in range(2)])
        x1t = T(es, "p7x1", [128, D]); x2 = T(es, "p7x2", [128, D])
        junk = T(es, "p7junk", [128, D], BF16)
        ss = T(es, "p7ss", [128, 1]); rstd = T(es, "p7rstd", [128, 1])
        yo = Rot([T(es, "p7yo%d" % i, [128, D]) for i in range(2)])
        Pa = Rot([P(es, "p7pa%d" % i, [128, 512]) for i in range(2)])
        Pg = Rot([P(es, "p7pg%d" % i, [128, 512]) for i in range(2)])
        Py = Rot([P(es, "p7py%d" % i, [128, 1024]) for i in range(2)])

        for tk in range(NTILE_PEER):
            for b in range(4):
                n = 4 * tk + b
                dma("sp", h2t[:, :, b * 128:(b + 1) * 128], h2T_d[n], [("h2T", n)], [h2t], h2t)
                dma("sp", rf[:, b, :, :, :], rt_f_d[n], [("rtf", n)], [rf], rf)
                dma("sp", kp[:, b, :], kap_d[n], [("kap", n)], [kp], kp)
            for b in range(4):
                for h in range(8):
                    ts("dve", dg[:, b, h, :], identb[:, :], kp[:, b, h:h + 1], None, ALU.mult, None,
                       [identb, kp], [dg])
            NI = NEXP_BLK
            grp = {}

            def get_group(ig):
                if ig not in grp:
                    uTg = uTg_r.next(); vBg = vBg_r.next(); ga2 = ga2_r.next()
                    i0 = ig * IG
                    dma("sp", uTg[:, :, :], uT_d[i0:i0 + IG].rearrange("i d k e -> d i (k e)"),
                        [("uT", i) for i in range(i0, i0 + IG)], [uTg], uTg)
                    dma("sp", vBg[:, :, :], vB_d[i0:i0 + IG].rearrange("i e d -> e i d"),
                        [("vB", i) for i in range(i0, i0 + IG)], [vBg], vBg)
                    grp[ig] = (uTg, vBg, ga2)
                return grp[ig]

            stU = {}

            def emit_U(i):
                uTg, vBg, ga2 = get_group(i // IG)
                ii = i % IG
                pa = Pa.next(); ga = ga_r.next()
                for kc in range(8):
                    mm(pa[:, :], uTg[:, ii, kc * 128:(kc + 1) * 128], h2t[:, kc, :],
                       kc == 0, kc == 7, [uTg, h2t], [pa])
                act(ga[:, :], pa[:, :], AF.Gelu_apprx_tanh, [pa], [ga])
                stU[i] = ga

            def emit_gate(i):
                eD = eD_r.next(); r1 = r1_r.next()
                slot = id(eD)
                keys = []
                for b in range(4):
                    for h in range(8):
                        if b == 3 or (b == 2 and h >= 4):
                            continue
                        kk = ("eD", slot, b, h)
                        keys.append(kk)
                        act(eD[:, b, h, :], rf[:, b, 0, h, :], AF.Copy, [rf], [kk],
                            scale=rf[:, b, 1, h, i:i + 1])
                kk = ("eD", slot, "p2")
                keys.append(kk)
                tt("pool", eD[:, 2, 4:8, :], rf[:, 2, 0, 4:8, :],
                   rf[:, 2, 1, 4:8, i:i + 1].to_broadcast([128, 4, 128]), ALU.mult, [rf], [kk])
                kk = ("eD", slot, "p3")
                keys.append(kk)
                tt("pool", eD[:, 3, :, :], rf[:, 3, 0, :, :],
                   rf[:, 3, 1, :, i:i + 1].to_broadcast([128, 8, 128]), ALU.mult, [rf], [kk])
                stt("dve", r1[:, :, :, :], eD[:, :, :, :], 1.0, eD[:, :, :, :], ALU.is_ge,
                    ALU.mult, keys, [r1])
                return r1

            def emit_diag(i, r1):
                uTg, vBg, ga2 = get_group(i // IG)
                ii = i % IG
                pgt = Pg.next()
                for b in range(4):
                    for h in range(8):
                        mm(pgt[:, b * 128:(b + 1) * 128], r1[:, b, h, :], dg[:, b, h, :],
                           h == 0, h == 7, [r1, dg], [pgt])
                ga = stU.pop(i)
                tt("dve", ga2[:, ii, :], pgt[:, :], ga[:, :], ALU.mult, [pgt, ga], [ga2])

            def emit_V(ig):
                uTg, vBg, ga2 = grp.pop(ig)
                for b in range(4):
                    py = Py.next()
                    for ii in range(IG):
                        for nh in range(2):
                            mm(py[:, nh * 512:(nh + 1) * 512], ga2[:, ii, b * 128:(b + 1) * 128],
                               vBg[:, ii, nh * 512:(nh + 1) * 512], ii == 0, ii == IG - 1,
                               [ga2, vBg], [py])
                    if ig == 0:
                        cp("dve", yacc[:, b, :], py[:, :], [py], [yacc])
                    else:
                        tt("dve", yacc[:, b, :], py[:, :], yacc[:, b, :], ALU.add, [py, yacc], [yacc])

            emit_U(0)
            for i in range(NI):
                r1 = emit_gate(i)
                if i + 1 < NI:
                    emit_U(i + 1)
                emit_diag(i, r1)
                if i % IG == IG - 1:
                    emit_V(i // IG)
            for b in range(4):
                n = 4 * tk + b
                y_t = yo.next()
                dma("sp", x1t[:, :], x1_d[n], [("x1", n)], [x1t], x1t)
                tt("dve", x2[:, :], yacc[:, b, :], GT2b[:, :], ALU.mult, [yacc, GT2b], [x2])
                tt("dve", x2[:, :], x2[:, :], x1t[:, :], ALU.add, [x2, x1t], [x2])
                act(junk[:, :], x2[:, :], AF.Square, [x2], [junk, ss], accum=ss[:, :])
                act(rstd[:, :], ss[:, :], AF.Sqrt, [ss, epsT], [rstd], bias=epsT[:, :], scale=1.0 / D)
                S.op("dve", lambda e: e.reciprocal(out=rstd[:, :], in_=rstd[:, :]), [rstd], [rstd])
                stt("dve", y_t[:, :], x2[:, :], rstd[:, 0:1], GFb[:, :], ALU.mult, ALU.mult,
                    [x2, rstd, GFb], [y_t])
                dma("pool", y_out[n * 128:(n + 1) * 128, :], y_t[:, :], [y_t], [], y_t)

    top.close()
    build_nc.stats = {e: len(S.q[e]) for e in ENGS}
    S.emit()
    return nc


def _consts():
    a = np.arange(128)
    ident = np.eye(128, dtype=np.float32)
    tri_incl = (a[:, None] >= a[None, :]).astype(np.float32)
    tri_low = (a[:, None] < a[None, :]).astype(np.float32)
    diagmask = (a[:, None] < a[None, :]).astype(np.float32)
    masks = []
    for r, nkb in DIL:
        for dl in range(nkb):
            d = 128 * dl + a[None, :] - a[:, None]
            masks.append(((d >= 0) & (d % r == 0) & (d <= 128 * r)).astype(np.float32))
    dilmask = np.stack(masks, axis=1)
    return ident, tri_incl, tri_low, diagmask, np.ascontiguousarray(dilmask)


def _rope_tables(shift):
    half = 32
    inv = (10000.0 ** (-np.arange(half, dtype=np.float32) / half)).astype(np.float32)
    pos = (np.arange(SEQ) - shift).astype(np.float32)
    ang = pos[:, None] * inv[None, :]
    cos = np.cos(ang).astype(np.float32).T
    sin = np.sin(ang).astype(np.float32).T
    C64 = np.concatenate([cos, cos], axis=0)
    S64 = np.concatenate([-sin, sin], axis=0)
    return (np.ascontiguousarray(np.concatenate([C64, C64], axis=0)),
            np.ascontiguousarray(np.concatenate([S64, S64], axis=0)))


def make_in_maps(x, c, w_ada, b_ada, g_mix, w_in, w_sb_o, w_dil_o, w_out, g_ffn, w_pq,
                 peer_keys, peer_u, peer_v, g_final):
    f = lambda a: np.ascontiguousarray(np.asarray(a, dtype=np.float32))
    x = f(x); c = f(c)
    w_in0 = f(w_in[0])
    perm = np.arange(1536)
    hd = perm % 64
    perm = perm - hd + (hd + 32) % 64
    w_perm = np.ascontiguousarray(w_in0[:, 1536:3072][:, perm])
    keysT = np.ascontiguousarray(
        np.transpose(f(peer_keys[0]).reshape(16, 128, 128), (2, 0, 1)))
    ident, tri_incl, tri_low, diagmask, dilmask = _consts()
    shared = {
        "w_ada": f(w_ada[0]), "b_ada": f(b_ada[0]).reshape(1, -1),
        "g_mix": f(g_mix[0]).reshape(1, -1), "g_ffn": f(g_ffn[0]).reshape(1, -1),
        "g_final": f(g_final).reshape(1, -1),
        "w_in": w_in0, "w_perm": w_perm, "w_sb_o": f(w_sb_o[0]), "w_dil_o": f(w_dil_o[0]),
        "w_out": f(w_out[0]), "w_pq": f(w_pq[0]), "keysT": keysT,
        "peer_u": f(peer_u[0]), "peer_v": f(peer_v[0]),
        "ident": ident, "tri_incl": tri_incl, "tri_low": tri_low, "diagmask": diagmask,
        "dilmask": dilmask,
    }
    ropes = {hh: _rope_tables(128 * (1 - hh)) for hh in (0, 1)}
    in_maps = []
    for core in range(8):
        b, hh = core // 2, core % 2
        if hh == 1:
            xall = x[b]
        else:
            xall = np.concatenate([np.zeros((128, D), np.float32), x[b, :SEQ - 128]], axis=0)
        m = dict(shared)
        m["xall"] = np.ascontiguousarray(xall)
        m["cT"] = np.ascontiguousarray(c[b].reshape(8, 128).T)
        m["kvalid"] = np.full((128, 1), float(hh), np.float32)
        m["ropeC"], m["ropeS"] = ropes[hh]
        in_maps.append(m)
    return in_maps


def kernel(**inputs):
    in_maps = make_in_maps(**inputs)
    nc = build_nc()
    res = run_bass_kernel_spmd(nc, in_maps, core_ids=list(range(8)))
    out = np.zeros((4, SEQ, D), np.float32)
    for core in range(8):
        b, hh = core // 2, core % 2
        y = np.asarray(res.results[core]["y"]).reshape(NOWN, 128, D)
        out[b].reshape(NB, 128, D)[hh::2] = y
    return out
```

```python
import contextlib
import numpy as np
import concourse.bass as bass
import concourse.mybir as mybir
from concourse.bass_utils import run_bass_kernel_spmd

F32 = mybir.dt.float32
BF16 = mybir.dt.bfloat16
AF = mybir.ActivationFunctionType
ALU = mybir.AluOpType
AX = mybir.AxisListType

D = 1024
SEQ = 8192
NB = 64
NOWN = 32
EPS = 1e-6
ENGS = ("pe", "act", "dve", "pool", "sp")
DIL = ((1, 2), (4, 5), (16, 17))
SAME_ENGINE_SYNC = True
DBG_SKIP = set()


class _Ins:
    __slots__ = ("eng", "fn", "waits", "signal", "sig_count", "is_dma", "dsem", "dcount")

    def __init__(self, eng, fn, is_dma=False):
        self.eng = eng
        self.fn = fn
        self.waits = []
        self.signal = False
        self.sig_count = 0
        self.is_dma = is_dma
        self.dsem = None
        self.dcount = 0


class Sched:
    def __init__(self, nc):
        self.nc = nc
        self.q = {e: [] for e in ENGS}
        self.last_w = {}
        self.readers = {}
        self.dma_counts = {}

    @staticmethod
    def _key(r):
        return r if isinstance(r, (tuple, str, int)) else id(r)

    def _dep(self, ins, other):
        if other is None or other is ins:
            return
        if other.is_dma:
            ins.waits.append(("D", other.dsem, other.dcount))
        elif other.eng != ins.eng or (SAME_ENGINE_SYNC and ins.eng != "pe"):
            other.signal = True
            ins.waits.append(("E", other.eng, other))

    def _track(self, ins, reads, writes):
        for r in reads:
            self._dep(ins, self.last_w.get(self._key(r)))
        for w in writes:
            k = self._key(w)
            self._dep(ins, self.last_w.get(k))
            rd = self.readers.get(k)
            if rd:
                for o in rd[0].values():
                    self._dep(ins, o)
                for o in rd[1]:
                    self._dep(ins, o)
        for r in reads:
            rd = self.readers.setdefault(self._key(r), ({}, []))
            if ins.is_dma:
                rd[1].append(ins)
            else:
                rd[0][ins.eng] = ins
        for w in writes:
            k = self._key(w)
            self.last_w[k] = ins
            self.readers[k] = ({}, [])

    def op(self, eng, fn, reads=(), writes=()):
        ins = _Ins(eng, fn)
        self._track(ins, reads, writes)
        self.q[eng].append(ins)
        return ins

    def dma(self, eng, fn, reads=(), writes=(), sem=None):
        ins = _Ins(eng, fn, is_dma=True)
        k = self._key(sem)
        self.dma_counts[k] = self.dma_counts.get(k, 0) + 16
        ins.dsem = k
        ins.dcount = self.dma_counts[k]
        self._track(ins, reads, writes)
        self.q[eng].append(ins)
        return ins

    def barrier(self):
        dsnap = dict(self.dma_counts)
        lastc = {}
        for f in ENGS:
            j = len(self.q[f]) - 1
            while j >= 0 and (self.q[f][j].is_dma or self.q[f][j].fn is None):
                j -= 1
            lastc[f] = self.q[f][j] if j >= 0 else None
        for e in ENGS:
            ins = _Ins(e, None)
            for f in ENGS:
                if f != e and lastc[f] is not None:
                    lastc[f].signal = True
                    ins.waits.append(("E", f, lastc[f]))
            for k, c in dsnap.items():
                ins.waits.append(("D", k, c))
            self.q[e].append(ins)
        self.last_w = {}
        self.readers = {}

    def emit(self):
        nc = self.nc
        for e in ENGS:
            c = 0
            for ins in self.q[e]:
                if ins.signal:
                    c += 1
                    ins.sig_count = c
        dkeys = list(self.dma_counts.keys())
        with contextlib.ExitStack() as es:
            esem = {e: es.enter_context(nc.semaphore("s_" + e)) for e in ENGS}
            dsem = {k: es.enter_context(nc.semaphore("d%d" % i)) for i, k in enumerate(dkeys)}
            block = es.enter_context(nc.Block())
            final_d = dict(self.dma_counts)

            def make(e):
                def body(engobj):
                    known_e = {f: 0 for f in ENGS}
                    known_d = {}
                    for ins in self.q[e]:
                        for w in ins.waits:
                            if w[0] == "E":
                                f, c = w[1], w[2].sig_count
                                if known_e[f] >= c:
                                    continue
                                known_e[f] = c
                                engobj.wait_ge(esem[f], c)
                            else:
                                k, c = w[1], w[2]
                                if known_d.get(k, 0) >= c:
                                    continue
                                known_d[k] = c
                                engobj.wait_ge(dsem[k], c)
                        if ins.fn is None:
                            continue
                        r = ins.fn(engobj)
                        if ins.is_dma:
                            r.then_inc(dsem[ins.dsem], 16)
                        elif ins.signal:
                            r.then_inc(esem[e], 1)
                    if e == "sp":
                        for k, c in final_d.items():
                            if known_d.get(k, 0) < c:
                                engobj.wait_ge(dsem[k], c)
                return body

            block.tensor(make("pe"))
            block.scalar(make("act"))
            block.vector(make("dve"))
            block.gpsimd(make("pool"))
            block.sync(make("sp"))


class Rot:
    def __init__(self, tiles):
        self.tiles = tiles
        self.i = 0

    def next(self):
        t = self.tiles[self.i % len(self.tiles)]
        self.i += 1
        return t


def build_nc(debug=None, NPAIR_SB=4, NPAIR_DIL=2, NEXP_BLK=128, NTILE_PEER=8, NTT=16, NQT=8, NOWN_EFF=32):
    nc = bass.Bass("TRN2", target_bir_lowering=False)
    S = Sched(nc)

    def din(name, shape, dt=F32):
        return nc.dram_tensor(name, list(shape), dt, kind="ExternalInput").ap()

    xall = din("xall", [SEQ, D])
    cT = din("cT", [128, 8])
    kvalid_d = din("kvalid", [128, 1])
    ropeC_d = din("ropeC", [128, SEQ])
    ropeS_d = din("ropeS", [128, SEQ])
    w_ada = din("w_ada", [D, 6 * D])
    b_ada = din("b_ada", [1, 6 * D])
    gmix_d = din("g_mix", [1, D])
    gffn_d = din("g_ffn", [1, D])
    gfin_d = din("g_final", [1, D])
    w_in = din("w_in", [D, 5888])
    w_perm = din("w_perm", [D, 1536])
    w_sb_o = din("w_sb_o", [512, D])
    w_dil_o = din("w_dil_o", [256, D])
    w_out = din("w_out", [D, D])
    w_pq = din("w_pq", [D, 2048])
    keysT_d = din("keysT", [128, 16, 128])
    peer_u = din("peer_u", [16384, D])
    peer_v = din("peer_v", [16384, D])
    ident_d = din("ident", [128, 128])
    triI_d = din("tri_incl", [128, 128])
    triL_d = din("tri_low", [128, 128])
    dmask_d = din("diagmask", [128, 128])
    dilm_d = din("dilmask", [128, 24, 128])

    y_out = nc.dram_tensor("y", [NOWN * 128, D], F32, kind="ExternalOutput").ap()
    dbg_out = None
    if debug:
        dbg_out = nc.dram_tensor("dbg", [128, 32768], F32, kind="ExternalOutput").ap()

    def dscr(name, shape, dt):
        return nc.dram_tensor(name, list(shape), dt, kind="Internal").ap()

    hT_d = dscr("hT_scr", [16, 128, 8, 512], BF16)
    x1_d = dscr("x1_scr", [NOWN, 128, D], F32)
    uT_d = dscr("uT_scr", [128, 128, 8, 128], BF16)
    vB_d = dscr("vB_scr", [128, 128, D], BF16)
    rt_f_d = dscr("rt_f", [NOWN, 128, 2, 8, 128], F32)
    kap_d = dscr("kap", [NOWN, 128, 8], F32)
    h2T_d = dscr("h2T_scr", [NOWN, 128, 8, 128], BF16)

    w_in_r = w_in.rearrange("(k p) n -> p k n", p=128)
    w_perm_r = w_perm.rearrange("(k p) n -> p k n", p=128)

    top = contextlib.ExitStack()

    def T(es, name, shape, dt=F32):
        return es.enter_context(nc.sbuf_tensor(name, list(shape), dt))

    def P(es, name, shape, dt=F32):
        return es.enter_context(nc.psum_tensor(name, list(shape), dt))

    def mm(out, lhsT, rhs, start, stop, reads, writes):
        S.op("pe", lambda e: e.matmul(out, lhsT=lhsT, rhs=rhs, start=start, stop=stop),
             reads, writes)

    def tr(out, in_, ident, reads, writes):
        S.op("pe", lambda e: e.transpose(out=out, in_=in_, identity=ident), reads, writes)

    def act(out, in_, func, reads, writes, bias=None, scale=None, accum=None):
        def f(e):
            kw = {}
            if bias is not None:
                kw["bias"] = bias
            if scale is not None:
                kw["scale"] = scale
            if accum is not None:
                kw["accum_out"] = accum
            return e.activation(out=out, in_=in_, func=func, **kw)
        S.op("act", f, reads, writes)

    def tt(eng, out, in0, in1, op, reads, writes):
        S.op(eng, lambda e: e.tensor_tensor(out=out, in0=in0, in1=in1, op=op), reads, writes)

    def ts(eng, out, in0, s1, s2, op0, op1, reads, writes):
        if s2 is None:
            S.op(eng, lambda e: e.tensor_scalar(out=out, in0=in0, scalar1=s1, scalar2=None,
                                                op0=op0), reads, writes)
        else:
            S.op(eng, lambda e: e.tensor_scalar(out=out, in0=in0, scalar1=s1, scalar2=s2,
                                                op0=op0, op1=op1), reads, writes)

    def stt(eng, out, in0, scalar, in1, op0, op1, reads, writes):
        S.op(eng, lambda e: e.scalar_tensor_tensor(out=out, in0=in0, scalar=scalar, in1=in1,
                                                   op0=op0, op1=op1), reads, writes)

    def cp(eng, out, in_, reads, writes):
        S.op(eng, lambda e: e.tensor_copy(out=out, in_=in_), reads, writes)

    def dma(eng, out, in_, reads, writes, sem):
        S.dma(eng, lambda e: e.dma_start(out=out, in_=in_), reads, writes, sem)

    identb = T(top, "identb", [128, 128], BF16)
    identf = T(top, "identf", [128, 128], F32)
    onesb = T(top, "onesb", [128, 128], BF16)
    onesf = T(top, "onesf", [128, 128], F32)
    kval = T(top, "kval", [128, 1], F32)
    epsT = T(top, "epsT", [128, 1], F32)
    G2b = T(top, "G2b", [128, D])
    SH2b = T(top, "SH2b", [128, D])
    GT1b = T(top, "GT1b", [128, D])
    GT2b = T(top, "GT2b", [128, D])
    GFb = T(top, "GFb", [128, D])
    es_attn = contextlib.ExitStack()
    osbT = T(es_attn, "osbT", [128, 4, NOWN * 128], BF16)
    odT = T(es_attn, "odT", [128, 2, NOWN * 128], BF16)
    es01 = contextlib.ExitStack()
    G1b = T(es01, "G1b", [128, D])
    SH1b = T(es01, "SH1b", [128, D])

    with contextlib.ExitStack() as es:
        scT = T(es, "scT", [128, 8])
        scB = T(es, "scB", [128, 8, 128])
        wad = T(es, "wad", [128, 8, 1536])
        modb = T(es, "modb", [128, 6 * D])
        badab = T(es, "badab", [128, 6 * D])
        gtmp = T(es, "gtmp", [128, D])
        ps0 = [P(es, "ps0_%d" % i, [128, 512]) for i in range(3)]

        dma("sp", identf[:, :], ident_d, [], [identf], identf)
        dma("sp", kval[:, :], kvalid_d, [], [kval], kval)
        dma("sp", scT[:, :], cT, [], [scT], scT)
        dma("sp", badab[:, :], b_ada.partition_broadcast(128)[:, 0, :], [], [badab], badab)
        dma("sp", GFb[:, :], gfin_d.partition_broadcast(128)[:, 0, :], [], [GFb], GFb)
        cp("dve", identb[:, :], identf[:, :], [identf], [identb])
        S.op("dve", lambda e: e.memset(onesf[:, :], 1.0), [], [onesf])
        S.op("dve", lambda e: e.memset(onesb[:, :], 1.0), [], [onesb])
        S.op("dve", lambda e: e.memset(epsT[:, :], EPS), [], [epsT])
        act(scT[:, :], scT[:, :], AF.Silu, [scT], [scT])
        for kc in range(8):
            ts("dve", scB[:, kc, :], onesf[:, :], scT[:, kc:kc + 1], None, ALU.mult, None,
               [onesf, scT], [scB])
        w_ada_r = w_ada.rearrange("(k p) n -> p k n", p=128)
        for g in range(4):
            dma("sp", wad[:, :, :], w_ada_r[:, :, g * 1536:(g + 1) * 1536], [], [wad], wad)
            for j in range(3):
                for kc in range(8):
                    mm(ps0[j][:, :], scB[:, kc, :], wad[:, kc, j * 512:(j + 1) * 512],
                       kc == 0, kc == 7, [scB, wad], [ps0[j]])
                c0 = g * 1536 + j * 512
                tt("dve", modb[:, c0:c0 + 512], ps0[j][:, :], badab[:, c0:c0 + 512], ALU.add,
                   [ps0[j], badab], [modb])
        cp("dve", SH1b[:, :], modb[:, 0:D], [modb], [SH1b])
        cp("dve", GT1b[:, :], modb[:, 2 * D:3 * D], [modb], [GT1b])
        cp("dve", SH2b[:, :], modb[:, 3 * D:4 * D], [modb], [SH2b])
        cp("dve", GT2b[:, :], modb[:, 5 * D:6 * D], [modb], [GT2b])
        dma("sp", gtmp[:, :], gmix_d.partition_broadcast(128)[:, 0, :], [], [gtmp], gtmp)
        stt("dve", G1b[:, :], modb[:, D:2 * D], 1.0, gtmp[:, :], ALU.add, ALU.mult,
            [modb, gtmp], [G1b])
        dma("sp", gtmp[:, :], gffn_d.partition_broadcast(128)[:, 0, :], [G1b], [gtmp], gtmp)
        stt("dve", G2b[:, :], modb[:, 4 * D:5 * D], 1.0, gtmp[:, :], ALU.add, ALU.mult,
            [modb, gtmp], [G2b])
        S.barrier()

    def norm_mod_T(x_t, Gb, SHb, junk, ss, rstd, xn, hblk, pT, outT, res):
        act(junk[:, :], x_t[:, :], AF.Square, [x_t], [junk, ss], accum=ss[:, :])
        act(rstd[:, :], ss[:, :], AF.Sqrt, [ss, epsT], [rstd], bias=epsT[:, :], scale=1.0 / D)
        S.op("dve", lambda e: e.reciprocal(out=rstd[:, :], in_=rstd[:, :]), [rstd], [rstd])
        stt("dve", xn[:, :], x_t[:, :], rstd[:, 0:1], Gb[:, :], ALU.mult, ALU.mult,
            [x_t, rstd, Gb], [xn])
        tt("dve", hblk[:, :], xn[:, :], SHb[:, :], ALU.add, [xn, SHb], [hblk])
        for kc in range(8):
            tr(pT[:, kc, :], hblk[:, kc * 128:(kc + 1) * 128], identb[:, :],
               [hblk, identb], [pT])
        act(outT, pT[:, :, :], AF.Copy, [pT], [res])

    with contextlib.ExitStack() as es:
        xr = Rot([T(es, "p1x%d" % i, [128, D]) for i in range(3)])
        junk = T(es, "p1junk", [128, D], BF16)
        ssr = Rot([T(es, "p1ss%d" % i, [128, 1]) for i in range(2)])
        rsr = Rot([T(es, "p1rs%d" % i, [128, 1]) for i in range(2)])
        xnr = Rot([T(es, "p1xn%d" % i, [128, D]) for i in range(2)])
        hbr = Rot([T(es, "p1hb%d" % i, [128, D], BF16) for i in range(2)])
        hTr = Rot([T(es, "p1hT%d" % i, [128, 8, 512], BF16) for i in range(2)])
        pTr = Rot([P(es, "p1pT%d" % i, [128, 8, 128], BF16) for i in range(2)])
        for tt_i in range(NTT):
            hTt = hTr.next()
            for j in range(4):
                tb = tt_i * 4 + j
                x_t = xr.next()
                dma("sp", x_t[:, :], xall[tb * 128:(tb + 1) * 128, :], [], [x_t], x_t)
                norm_mod_T(x_t, G1b, SH1b, junk, ssr.next(), rsr.next(), xnr.next(), hbr.next(),
                           pTr.next(), hTt[:, :, j * 128:(j + 1) * 128], hTt)
            dma("pool", hT_d[tt_i], hTt[:, :, :], [hTt], [("hT", tt_i)], hTt)
        S.barrier()
    es01.close()

    def load_w_bf16(dst, src_ap, n, stage_rot, eng="dve", c_off=0):
        step = 128
        for c0 in range(0, n, step):
            st = stage_rot.next()
            dma("sp", st[:, :, :], src_ap[:, :, c0:c0 + step], [], [st], st)
            cp(eng, dst[:, :, c_off + c0:c_off + c0 + step], st[:, :, :], [st], [dst])

    with contextlib.ExitStack() as es:
        stg = Rot([T(es, "p2stg%d" % i, [128, 8, 128]) for i in range(2)])
        Wq = T(es, "p2Wq", [128, 8, 128], BF16)
        Wk = T(es, "p2Wk", [128, 8, 128], BF16)
        Wv = T(es, "p2Wv", [128, 8, 128], BF16)
        KT = T(es, "p2KT", [128, SEQ], BF16)
        V = T(es, "p2V", [128, NB, 128], BF16)
        QT = T(es, "p2QT", [128, NOWN * 128], BF16)
        hTr = Rot([T(es, "p2hT%d" % i, [128, 8, 512], BF16) for i in range(2)])
        triI = T(es, "p2triI", [128, 128], BF16)
        triL = T(es, "p2triL", [128, 128], BF16)
        dmk = T(es, "p2dmk", [128, 128], F32)
        tmpf = T(es, "p2tmpf", [128, 128], F32)
        Et = [Rot([T(es, "p2E%d_%d" % (h, i), [128, 512]) for i in range(2)]) for h in range(2)]
        Lt = [Rot([T(es, "p2L%d_%d" % (h, i), [128, 512], BF16) for i in range(2)]) for h in range(2)]
        EXt = [Rot([T(es, "p2X%d_%d" % (h, i), [128, 512]) for i in range(2)]) for h in range(2)]
        At = [Rot([T(es, "p2A%d_%d" % (h, i), [128, 512], BF16) for i in range(2)]) for h in range(2)]
        Zb4 = [P(es, "p2Z%d" % i, [128, 512]) for i in range(4)]
        Zp = [Rot(Zb4[0:2]), Rot(Zb4[2:4])]
        Cp = [P(es, "p2C%d" % h, [128, 512]) for h in range(2)]
        Op = [P(es, "p2O%d" % h, [128, 512]) for h in range(2)]
        Gp = Rot(Zb4)

        dma("sp", tmpf[:, :], triI_d, [], [tmpf], tmpf)
        cp("dve", triI[:, :], tmpf[:, :], [tmpf], [triI])
        dma("sp", tmpf[:, :], triL_d, [triI], [tmpf], tmpf)
        cp("dve", triL[:, :], tmpf[:, :], [tmpf], [triL])
        dma("sp", dmk[:, :], dmask_d, [], [dmk], dmk)

        if NPAIR_SB == 0:
            for pr in range(4):
                S.op("pool", lambda e, pr=pr: e.memset(osbT[:, pr, :], 0.0), [],
                     [("osbT", pr, q) for q in range(8)])
        for pair in range(NPAIR_SB):
            load_w_bf16(Wq, w_in_r[:, :, pair * 128:(pair + 1) * 128], 128, stg)
            load_w_bf16(Wk, w_in_r[:, :, 512 + pair * 128:512 + (pair + 1) * 128], 128, stg)
            load_w_bf16(Wv, w_in_r[:, :, 1024 + pair * 128:1024 + (pair + 1) * 128], 128, stg)
            for tt_i in range(NTT):
                hTt = hTr.next()
                dma("sp", hTt[:, :, :], hT_d[tt_i], [("hT", tt_i)], [hTt], hTt)
                g = Gp.next()
                for kc in range(8):
                    mm(g[:, :], Wk[:, kc, :], hTt[:, kc, :], kc == 0, kc == 7, [Wk, hTt], [g])
                cp("dve", KT[:, tt_i * 512:(tt_i + 1) * 512], g[:, :], [g], [("KT", tt_i)])
                g = Gp.next()
                for j in range(4):
                    for kc in range(8):
                        mm(g[:, j * 128:(j + 1) * 128], hTt[:, kc, j * 128:(j + 1) * 128],
                           Wv[:, kc, :], kc == 0, kc == 7, [Wv, hTt], [g])
                act(V[:, tt_i * 4:(tt_i + 1) * 4, :], g[:, :].rearrange("p (j c) -> p j c", j=4),
                    AF.Copy, [g], [("V", tt_i)])
                g = Gp.next()
                for jj, j in enumerate((1, 3)):
                    for kc in range(8):
                        mm(g[:, jj * 128:(jj + 1) * 128], Wq[:, kc, :],
                           hTt[:, kc, j * 128:(j + 1) * 128], kc == 0, kc == 7, [Wq, hTt], [g])
                n0 = tt_i * 2
                act(QT[:, n0 * 128:(n0 + 2) * 128], g[:, 0:256], AF.Copy, [g], [("QT", n0 // 4)],
                    scale=0.125)
            for qi in range(NQT):
                kb_top = 8 * qi + 7
                qres = ("QT", qi)
                st = {}

                def stage1_pe(kb, h):
                    jmin = max(0, (kb - 8 * qi) // 2)
                    c0 = jmin * 128
                    r0, r1 = h * 64, (h + 1) * 64
                    Z = Zp[h].next()
                    st[(kb, h)] = dict(c0=c0, Z=Z, diag=(kb == 8 * qi + 2 * jmin + 1))
                    mm(Z[:, c0:512], KT[r0:r1, kb * 128:(kb + 1) * 128],
                       QT[r0:r1, qi * 512 + c0:(qi + 1) * 512], True, True,
                       [("KT", kb // 4), qres], [Z])

                def stage1_el(kb, h):
                    d = st[(kb, h)]
                    c0, Z = d["c0"], d["Z"]
                    E = Et[h].next(); L = Lt[h].next()
                    d["E"], d["L"] = E, L
                    act(E[:, c0:512], Z[:, c0:512], AF.Exp, [Z], [E])
                    if d["diag"]:
                        tt("dve", E[:, c0:c0 + 128], E[:, c0:c0 + 128], dmk[:, :], ALU.mult,
                           [E, dmk], [E])
                    if kb == 0:
                        ts("dve", E[:, c0:512], E[:, c0:512], kval[:, 0:1], None, ALU.mult,
                           None, [E, kval], [E])
                    act(L[:, c0:512], E[:, c0:512], AF.Ln, [E], [L], bias=1.0)

                def c1(kb, h):
                    d = st[(kb, h)]
                    c0 = d["c0"]
                    mm(Cp[h][:, c0:512], triI[:, :], d["L"][:, c0:512], kb == kb_top, True,
                       [triI, d["L"]], [Cp[h]])

                def exa(kb, h):
                    d = st[(kb, h)]
                    c0 = d["c0"]
                    EX = EXt[h].next(); A = At[h].next()
                    d["A"] = A
                    act(EX[:, c0:512], Cp[h][:, c0:512], AF.Exp, [Cp[h]], [EX], scale=-1.0)
                    tt("dve", A[:, c0:512], d["E"][:, c0:512], EX[:, c0:512], ALU.mult,
                       [d["E"], EX], [A])

                def oc2(kb, h):
                    d = st[(kb, h)]
                    c0 = d["c0"]
                    mm(Op[h][:, c0:512], V[:, kb, :], d["A"][:, c0:512], kb == kb_top, kb == 0,
                       [("V", kb // 4), d["A"]], [Op[h]])
                    if kb > 0:
                        mm(Cp[h][:, c0:512], triL[:, :], d["L"][:, c0:512], False, False,
                           [triL, d["L"]], [Cp[h]])

                for h in range(2):
                    stage1_pe(kb_top, h)
                for h in range(2):
                    stage1_el(kb_top, h)
                for kb in range(kb_top, -1, -1):
                    for h in range(2):
                        c1(kb, h)
                    if kb > 0:
                        for h in range(2):
                            stage1_pe(kb - 1, h)
                    for h in range(2):
                        exa(kb, h)
                    if kb > 0:
                        for h in range(2):
                            stage1_el(kb - 1, h)
                    for h in range(2):
                        oc2(kb, h)
                for h in range(2):
                    r0, r1 = h * 64, (h + 1) * 64
                    act(osbT[r0:r1, pair, qi * 512:(qi + 1) * 512], Op[h][r0:r1, :], AF.Copy,
                        [Op[h]], [("osbT", pair, qi)])
        S.barrier()

    if debug == "sb":
        with contextlib.ExitStack() as es:
            st = Rot([T(es, "dbgst%d" % i, [128, 2048]) for i in range(2)])
            for pr in range(4):
                for c in range(2):
                    s_ = st.next()
                    cp("dve", s_[:, :], osbT[:, pr, c * 2048:(c + 1) * 2048],
                       [("osbT", pr, q) for q in range(8)], [s_])
                    dma("sp", dbg_out[:, pr * 4096 + c * 2048: pr * 4096 + (c + 1) * 2048], s_[:, :],
                        [s_], [], s_)
        es_attn.close(); top.close()
        S.emit()
        return nc

    with contextlib.ExitStack() as es:
        stg = Rot([T(es, "p3stg%d" % i, [128, 8, 128]) for i in range(2)])
        Wq = T(es, "p3Wq", [128, 8, 128], BF16)
        Wk = T(es, "p3Wk", [128, 8, 128], BF16)
        Wv = T(es, "p3Wv", [128, 8, 128], BF16)
        Wqp = T(es, "p3Wqp", [128, 8, 128], BF16)
        Wkp = T(es, "p3Wkp", [128, 8, 128], BF16)
        KT = T(es, "p3KT", [128, SEQ], BF16)
        V = T(es, "p3V", [128, NB, 128], BF16)
        QT = T(es, "p3QT", [128, NOWN * 256], BF16)
        hTr = Rot([T(es, "p3hT%d" % i, [128, 8, 512], BF16) for i in range(2)])
        Cr = Rot([T(es, "p3C%d" % i, [128, 512]) for i in range(2)])
        Sr = Rot([T(es, "p3S%d" % i, [128, 512]) for i in range(2)])
        t1r = Rot([T(es, "p3t1%d" % i, [128, 512]) for i in range(2)])
        t2r = Rot([T(es, "p3t2%d" % i, [128, 512]) for i in range(2)])
        dilm = T(es, "p3dilm", [128, 24, 128], BF16)
        Er = Rot([T(es, "p3E%d" % i, [128, 256], BF16) for i in range(3)])
        Oacc = T(es, "p3Oacc", [128, NOWN * 128], F32)
        Zacc = T(es, "p3Zacc", [128, NOWN * 128], F32)
        Gp = Rot([P(es, "p3G%d" % i, [128, 512]) for i in range(4)])
        Zr = Rot([P(es, "p3Z%d" % i, [128, 512]) for i in range(2)])
        Ob = P(es, "p3Ob", [128, 512])
        Zs = P(es, "p3Zs", [128, 512])

        for m0 in range(0, 24, 8):
            st = stg.next()
            dma("sp", st[:, :, :], dilm_d[:, m0:m0 + 8, :], [], [st], st)
            cp("dve", dilm[:, m0:m0 + 8, :], st[:, :, :], [st], [dilm])
        mbase = (0, 2, 7)
        S.op("pool", lambda e: e.memset(QT[:, :], 0.0), [], [("QT", q) for q in range(8)])
        PENG = "dve" if "p" in DBG_SKIP else "pool"
        for sp_i in range(NPAIR_DIL):
            for g, (r, nkb) in enumerate(DIL):
                hc = (g * 4 + 2 * sp_i) * 64
                load_w_bf16(Wq, w_in_r[:, :, 1536 + hc:1536 + hc + 128], 128, stg)
                load_w_bf16(Wk, w_in_r[:, :, 2304 + hc:2304 + hc + 128], 128, stg)
                load_w_bf16(Wv, w_in_r[:, :, 3072 + hc:3072 + hc + 128], 128, stg)
                load_w_bf16(Wqp, w_perm_r[:, :, hc:hc + 128], 128, stg)
                load_w_bf16(Wkp, w_perm_r[:, :, 768 + hc:768 + hc + 128], 128, stg)
                for tt_i in range(NTT):
                    hTt = hTr.next(); Ct = Cr.next(); St = Sr.next()
                    dma("sp", hTt[:, :, :], hT_d[tt_i], [("hT", tt_i)], [hTt], hTt)
                    dma("sp", Ct[:, :], ropeC_d[:, tt_i * 512:(tt_i + 1) * 512], [], [Ct], Ct)
                    dma("sp", St[:, :], ropeS_d[:, tt_i * 512:(tt_i + 1) * 512], [], [St], St)
                    g1 = Gp.next(); g2 = Gp.next(); t1 = t1r.next(); t2 = t2r.next()
                    for kc in range(8):
                        mm(g1[:, :], Wk[:, kc, :], hTt[:, kc, :], kc == 0, kc == 7, [Wk, hTt], [g1])
                    for kc in range(8):
                        mm(g2[:, :], Wkp[:, kc, :], hTt[:, kc, :], kc == 0, kc == 7, [Wkp, hTt], [g2])
                    tt("dve", t1[:, :], g1[:, :], Ct[:, :], ALU.mult, [g1, Ct], [t1])
                    tt("dve", t2[:, :], g2[:, :], St[:, :], ALU.mult, [g2, St], [t2])
                    tt(PENG, KT[:, tt_i * 512:(tt_i + 1) * 512], t1[:, :], t2[:, :], ALU.add,
                       [t1, t2], [("KT", tt_i)])
                    g3 = Gp.next()
                    for j in range(4):
                        for kc in range(8):
                            mm(g3[:, j * 128:(j + 1) * 128], hTt[:, kc, j * 128:(j + 1) * 128],
                               Wv[:, kc, :], kc == 0, kc == 7, [Wv, hTt], [g3])
                    act(V[:, tt_i * 4:(tt_i + 1) * 4, :], g3[:, :].rearrange("p (j c) -> p j c", j=4),
                        AF.Copy, [g3], [("V", tt_i)])
                    g1 = Gp.next(); g2 = Gp.next(); t1 = t1r.next(); t2 = t2r.next()
                    for jj, j in enumerate((1, 3)):
                        for kc in range(8):
                            mm(g1[:, jj * 128:(jj + 1) * 128], Wq[:, kc, :],
                               hTt[:, kc, j * 128:(j + 1) * 128], kc == 0, kc == 7, [Wq, hTt], [g1])
                        for kc in range(8):
                            mm(g2[:, jj * 128:(jj + 1) * 128], Wqp[:, kc, :],
                               hTt[:, kc, j * 128:(j + 1) * 128], kc == 0, kc == 7, [Wqp, hTt], [g2])
                    Cv = Ct[:, :].rearrange("p (j c) -> p j c", j=4)[:, 1::2, :]
                    Sv = St[:, :].rearrange("p (j c) -> p j c", j=4)[:, 1::2, :]
                    v3 = lambda ap: ap.rearrange("p (j c) -> p j c", j=2)
                    tt("dve", v3(t1[:, 0:256]), v3(g1[:, 0:256]), Cv, ALU.mult, [g1, Ct], [t1])
                    tt("dve", v3(t2[:, 0:256]), v3(g2[:, 0:256]), Sv, ALU.mult, [g2, St], [t2])
                    n0 = tt_i * 2
                    for h in range(2):
                        r0, r1 = h * 64, (h + 1) * 64
                        qv = QT[r0:r1, n0 * 256:(n0 + 2) * 256].rearrange(
                            "p (j c) -> p j c", j=2)[:, :, h * 128:(h + 1) * 128]
                        tt(PENG, qv, v3(t1[r0:r1, 0:256]), v3(t2[r0:r1, 0:256]), ALU.add,
                           [t1, t2], [("QT", n0 // 4)])
                flat = []
                for n in range(0 if "a" in DBG_SKIP else NOWN_EFF):
                    Pb = 2 * n + 1
                    steps = [dl for dl in range(nkb) if Pb - dl >= 0]
                    for si, dl in enumerate(steps):
                        flat.append((n, si, dl, len(steps)))
                stA = {}

                def stageA(k):
                    n, si, dl, ns = flat[k]
                    kb = 2 * n + 1 - dl
                    Zb = Zr.next(); E = Er.next()
                    mm(Zb[:, 0:256], KT[:, kb * 128:(kb + 1) * 128],
                       QT[:, n * 256:(n + 1) * 256], True, True,
                       [("KT", kb // 4), ("QT", n // 4)], [Zb])
                    act(E[:, :], Zb[:, 0:256], AF.Exp, [Zb], [E], scale=0.125)
                    mi = mbase[g] + dl
                    tt(PENG, E[:, :].rearrange("p (h q) -> p h q", h=2),
                       E[:, :].rearrange("p (h q) -> p h q", h=2),
                       dilm[:, mi:mi + 1, :].to_broadcast([128, 2, 128]), ALU.mult,
                       [E, dilm], [E])
                    if kb == 0:
                        ts("dve", E[:, :], E[:, :], kval[:, 0:1], None, ALU.mult, None,
                           [E, kval], [E])
                    stA[k] = E

                def stageB(k):
                    n, si, dl, ns = flat[k]
                    kb = 2 * n + 1 - dl
                    E = stA.pop(k)
                    mm(Ob[:, 0:256], V[:, kb, :], E[:, :], si == 0, si == ns - 1,
                       [("V", kb // 4), E], [Ob])
                    mm(Zs[:, 0:256], onesb[:, :], E[:, :], si == 0, si == ns - 1,
                       [onesb, E], [Zs])
                    if si == ns - 1:
                        for h in range(2):
                            r0, r1 = h * 64, (h + 1) * 64
                            oa = Oacc[r0:r1, n * 128:(n + 1) * 128]
                            za = Zacc[r0:r1, n * 128:(n + 1) * 128]
                            if g == 0:
                                cp("dve", oa, Ob[r0:r1, h * 128:(h + 1) * 128], [Ob], [Oacc])
                                cp("dve", za, Zs[r0:r1, h * 128:(h + 1) * 128], [Zs], [Zacc])
                            else:
                                tt("dve", oa, Ob[r0:r1, h * 128:(h + 1) * 128], oa, ALU.add,
                                   [Ob, Oacc], [Oacc])
                                tt("dve", za, Zs[r0:r1, h * 128:(h + 1) * 128], za, ALU.add,
                                   [Zs, Zacc], [Zacc])

                if flat:
                    stageA(0)
                for k in range(len(flat)):
                    if k + 1 < len(flat):
                        stageA(k + 1)
                    stageB(k)
            NE = NOWN_EFF * 128
            S.op("dve", lambda e: e.reciprocal(out=Zacc[:, 0:NE], in_=Zacc[:, 0:NE]), [Zacc], [Zacc])
            tt("dve", odT[:, sp_i, 0:NE], Oacc[:, 0:NE], Zacc[:, 0:NE], ALU.mult, [Oacc, Zacc],
               [("odT", sp_i)])
        S.barrier()

    if debug == "dil":
        with contextlib.ExitStack() as es:
            st = Rot([T(es, "dbgst%d" % i, [128, 2048]) for i in range(2)])
            for pr in range(2):
                for c in range(2):
                    s_ = st.next()
                    cp("dve", s_[:, :], odT[:, pr, c * 2048:(c + 1) * 2048], [("odT", pr)], [s_])
                    dma("sp", dbg_out[:, pr * 4096 + c * 2048: pr * 4096 + (c + 1) * 2048], s_[:, :],
                        [s_], [], s_)
        es_attn.close(); top.close()
        S.emit()
        return nc

    with contextlib.ExitStack() as es:
        stg = Rot([T(es, "p4stg%d" % i, [128, 8, 128]) for i in range(2)])
        Wg = T(es, "p4Wg", [128, 8, 2048], BF16)
        Wsbo = T(es, "p4Wsbo", [128, 4, 1024], BF16)
        Wdo = T(es, "p4Wdo", [128, 2, 1024], BF16)
        Wout = T(es, "p4Wout", [128, 8, 1024], BF16)
        hBr = Rot([T(es, "p4hB%d" % i, [128, 8, 128], BF16) for i in range(2)])
        sg = T(es, "p4sg", [128, 2048], BF16)
        m1 = T(es, "p4m1", [128, D])
        m2 = T(es, "p4m2", [128, D])
        mg = T(es, "p4mg", [128, D], BF16)
        mT = T(es, "p4mT", [128, 8, 128], BF16)
        xr = Rot([T(es, "p4x%d" % i, [128, D]) for i in range(2)])
        x1r = Rot([T(es, "p4x1%d" % i, [128, D]) for i in range(2)])
        tmpr = T(es, "p4tmp", [128, D])
        pg = [P(es, "p4pg%d" % i, [128, 512]) for i in range(4)]
        pbs = [P(es, "p4pbs%d" % i, [128, 512]) for i in range(2)]
        pbd = [P(es, "p4pbd%d" % i, [128, 512]) for i in range(2)]

        load_w_bf16(Wg, w_in_r[:, :, 3840:5888], 2048, stg)
        for t4 in range(4):
            for c0 in range(0, 1024, 512):
                st = stg.next()
                stv = st[:, :, :].rearrange("p a b -> p (a b)")
                dma("sp", stv[:, 0:512], w_sb_o[t4 * 128:(t4 + 1) * 128, c0:c0 + 512], [], [st], st)
                cp("dve", Wsbo[:, t4, c0:c0 + 512], stv[:, 0:512], [st], [Wsbo])
        for t2 in range(2):
            for c0 in range(0, 1024, 512):
                st = stg.next()
                stv = st[:, :, :].rearrange("p a b -> p (a b)")
                dma("sp", stv[:, 0:512], w_dil_o[t2 * 128:(t2 + 1) * 128, c0:c0 + 512], [], [st], st)
                cp("dve", Wdo[:, t2, c0:c0 + 512], stv[:, 0:512], [st], [Wdo])
        load_w_bf16(Wout, w_out.rearrange("(k p) n -> p k n", p=128), 1024, stg)

        for n in range(NOWN_EFF):
            tti, j = n // 2, 1 + 2 * (n % 2)
            hB = hBr.next()
            dma("sp", hB[:, :, :], hT_d[tti][:, :, j * 128:(j + 1) * 128], [("hT", tti)], [hB], hB)
            for c4 in range(4):
                for kc in range(8):
                    mm(pg[c4][:, :], hB[:, kc, :], Wg[:, kc, c4 * 512:(c4 + 1) * 512],
                       kc == 0, kc == 7, [hB, Wg], [pg[c4]])
                act(sg[:, c4 * 512:(c4 + 1) * 512], pg[c4][:, :], AF.Sigmoid, [pg[c4]], [sg])
            for nh in range(2):
                for t4 in range(4):
                    mm(pbs[nh][:, :], osbT[:, t4, n * 128:(n + 1) * 128],
                       Wsbo[:, t4, nh * 512:(nh + 1) * 512], t4 == 0, t4 == 3,
                       [("osbT", t4, n // 4), Wsbo], [pbs[nh]])
                for t2 in range(2):
                    mm(pbd[nh][:, :], odT[:, t2, n * 128:(n + 1) * 128],
                       Wdo[:, t2, nh * 512:(nh + 1) * 512], t2 == 0, t2 == 1,
                       [("odT", t2), Wdo], [pbd[nh]])
                tt("dve", m1[:, nh * 512:(nh + 1) * 512], pbs[nh][:, :],
                   sg[:, nh * 512:(nh + 1) * 512], ALU.mult, [pbs[nh], sg], [m1])
                tt("dve", m2[:, nh * 512:(nh + 1) * 512], pbd[nh][:, :],
                   sg[:, 1024 + nh * 512:1024 + (nh + 1) * 512], ALU.mult, [pbd[nh], sg], [m2])
            tt("pool", mg[:, :], m1[:, :], m2[:, :], ALU.add, [m1, m2], [mg])
            pTm = pg[0][:, :].bitcast(BF16).rearrange("p (k t) -> p k t", k=8)
            for kc in range(8):
                tr(pTm[:, kc, :], mg[:, kc * 128:(kc + 1) * 128], identb[:, :], [mg, identb], [pg[0]])
            act(mT[:, :, :], pTm, AF.Copy, [pg[0]], [mT])
            x_t = xr.next(); x1 = x1r.next()
            tb = 2 * n + 1
            dma("sp", x_t[:, :], xall[tb * 128:(tb + 1) * 128, :], [], [x_t], x_t)
            for nh in range(2):
                for kc in range(8):
                    mm(pg[1 + nh][:, :], mT[:, kc, :], Wout[:, kc, nh * 512:(nh + 1) * 512],
                       kc == 0, kc == 7, [mT, Wout], [pg[1 + nh]])
                tt("dve", tmpr[:, nh * 512:(nh + 1) * 512], pg[1 + nh][:, :],
                   GT1b[:, nh * 512:(nh + 1) * 512], ALU.mult, [pg[1 + nh], GT1b], [tmpr])
            tt("pool", x1[:, :], tmpr[:, :], x_t[:, :], ALU.add, [tmpr, x_t], [x1])
            dma("pool", x1_d[n], x1[:, :], [x1], [("x1", n)], x1)
            if debug == "x1":
                dma("pool", dbg_out[:, n * 1024:(n + 1) * 1024], x1[:, :], [x1], [], x1)
        S.barrier()
    es_attn.close()

    if debug == "x1":
        top.close()
        S.emit()
        return nc

    NEG = -1.0e30
    with contextlib.ExitStack() as es:
        stg = Rot([T(es, "p5stg%d" % i, [128, 8, 128]) for i in range(2)])
        Wpq = T(es, "p5Wpq", [128, 8, 2048], BF16)
        keysf = T(es, "p5keysf", [128, 16, 128], F32)
        keysb = T(es, "p5keysb", [128, 16, 128], BF16)
        xr = Rot([T(es, "p5x%d" % i, [128, D]) for i in range(2)])
        junk = T(es, "p5junk", [128, D], BF16)
        ss = T(es, "p5ss", [128, 1]); rstd = T(es, "p5rstd", [128, 1])
        xn = T(es, "p5xn", [128, D]); hblk = T(es, "p5hblk", [128, D], BF16)
        h2Br = Rot([T(es, "p5h2B%d" % i, [128, 8, 128], BF16) for i in range(2)])
        qT = T(es, "p5qT", [128, 16, 128], BF16)
        s_all = T(es, "p5sall", [128, 16, 128], F32)
        work = T(es, "p5work", [128, 16, 128], F32)
        top16 = T(es, "p5top16", [128, 16, 16], F32)
        cand = T(es, "p5cand", [128, 8, 256], F32)
        cw1 = T(es, "p5cw1", [128, 8, 256], F32)
        cw2 = T(es, "p5cw2", [128, 8, 256], F32)
        c24 = T(es, "p5c24", [128, 8, 24], F32)
        tau = T(es, "p5tau", [128, 8], F32)
        zt = T(es, "p5zt", [128, 8, 16], F32)
        Zsum = T(es, "p5Zsum", [128, 8], F32)
        d0 = T(es, "p5d0", [128, 8, 128], F32)
        e1 = T(es, "p5e1", [128, 8, 128], F32)
        rfr = Rot([T(es, "p5rf%d" % i, [128, 2, 8, 128], F32) for i in range(2)])
        tm8 = T(es, "p5tm8", [128, 8], F32)
        kpr = Rot([T(es, "p5kp%d" % i, [128, 8], F32) for i in range(2)])
        pT = P(es, "p5pT", [128, 8, 128], BF16)
        pq = [P(es, "p5pq%d" % i, [128, 512]) for i in range(4)]

        load_w_bf16(Wpq, w_pq.rearrange("(k p) n -> p k n", p=128), 2048, stg)
        dma("sp", keysf[:, :, :], keysT_d, [], [keysf], keysf)
        cp("dve", keysb[:, :, :], keysf[:, :, :], [keysf], [keysb])

        for n in range(NOWN_EFF):
            x_t = xr.next(); h2B = h2Br.next(); rf = rfr.next(); kp = kpr.next()
            dma("sp", x_t[:, :], x1_d[n], [("x1", n)], [x_t], x_t)
            norm_mod_T(x_t, G2b, SH2b, junk, ss, rstd, xn, hblk, pT, h2B[:, :, :], h2B)
            dma("pool", h2T_d[n], h2B[:, :, :], [h2B], [("h2T", n)], h2B)
            for hp in range(16):
                for kc in range(8):
                    mm(pq[hp // 4][:, (hp % 4) * 128:(hp % 4 + 1) * 128],
                       Wpq[:, kc, hp * 128:(hp + 1) * 128], h2B[:, kc, :], kc == 0, kc == 7,
                       [Wpq, h2B], [pq[hp // 4]])
            for b4 in range(4):
                act(qT[:, b4 * 4:(b4 + 1) * 4, :],
                    pq[b4][:, :].rearrange("p (a c) -> p a c", a=4), AF.Copy, [pq[b4]], [qT])
            for hp in range(16):
                mm(pq[hp // 4][:, (hp % 4) * 128:(hp % 4 + 1) * 128], qT[:, hp, :], keysb[:, hp, :],
                   True, True, [qT, keysb], [pq[hp // 4]])
            for b4 in range(4):
                cp("dve", s_all[:, b4 * 4:(b4 + 1) * 4, :],
                   pq[b4][:, :].rearrange("p (a c) -> p a c", a=4), [pq[b4]], [s_all])
            for hp in range(16):
                S.op("dve", lambda e, hp=hp: e.max(out=top16[:, hp, 0:8], in_=s_all[:, hp, :]),
                     [s_all], [top16])
                S.op("dve", lambda e, hp=hp: e.match_replace(
                    out=work[:, hp, :], in_to_replace=top16[:, hp, 0:8], in_values=s_all[:, hp, :],
                    imm_value=NEG), [s_all, top16], [work])
                S.op("dve", lambda e, hp=hp: e.max(out=top16[:, hp, 8:16], in_=work[:, hp, :]),
                     [work], [top16])
            t4v = top16[:, :, :].rearrange("p (h two) k -> p h two k", two=2)
            a0 = t4v[:, :, 0, :].unsqueeze(3).to_broadcast([128, 8, 16, 16])
            a1 = t4v[:, :, 1, :].unsqueeze(2).to_broadcast([128, 8, 16, 16])
            tt("dve", cand[:, :, :].rearrange("p h (a b) -> p h a b", a=16), a0, a1, ALU.add,
               [top16], [cand])
            for h in range(8):
                S.op("dve", lambda e, h=h: e.max(out=c24[:, h, 0:8], in_=cand[:, h, :]), [cand], [c24])
                S.op("dve", lambda e, h=h: e.match_replace(
                    out=cw1[:, h, :], in_to_replace=c24[:, h, 0:8], in_values=cand[:, h, :],
                    imm_value=NEG), [cand, c24], [cw1])
                S.op("dve", lambda e, h=h: e.max(out=c24[:, h, 8:16], in_=cw1[:, h, :]), [cw1], [c24])
                S.op("dve", lambda e, h=h: e.match_replace(
                    out=cw2[:, h, :], in_to_replace=c24[:, h, 8:16], in_values=cw1[:, h, :],
                    imm_value=NEG), [cw1, c24], [cw2])
                S.op("dve", lambda e, h=h: e.max(out=c24[:, h, 16:24], in_=cw2[:, h, :]), [cw2], [c24])
            tt("dve", tau[:, :], c24[:, :, 15], c24[:, :, 16], ALU.add, [c24], [tau])
            ts("dve", tau[:, :], tau[:, :], 0.5, None, ALU.mult, None, [tau], [tau])
            tt("dve", zt[:, :, :], c24[:, :, 0:16], c24[:, :, 0:1].to_broadcast([128, 8, 16]),
               ALU.subtract, [c24], [zt])
            act(zt[:, :, :], zt[:, :, :], AF.Exp, [zt], [zt])
            S.op("dve", lambda e: e.reduce_sum(out=Zsum[:, :], in_=zt[:, :, :], axis=AX.X),
                 [zt], [Zsum])
            S.op("dve", lambda e: e.reciprocal(out=Zsum[:, :], in_=Zsum[:, :]), [Zsum], [Zsum])
            s4 = s_all[:, :, :].rearrange("p (h two) j -> p h two j", two=2)
            s0v, s1v = s4[:, :, 0, :], s4[:, :, 1, :]
            m0v = t4v[:, :, 0, 0:1].to_broadcast([128, 8, 128])
            m1v = t4v[:, :, 1, 0:1].to_broadcast([128, 8, 128])
            tt("dve", d0[:, :, :], s1v, m1v, ALU.subtract, [s_all, top16], [d0])
            act(rf[:, 0, :, :], d0[:, :, :], AF.Exp, [d0], [rf])
            tt("dve", tm8[:, :], t4v[:, :, 1, 0], tau[:, :], ALU.subtract, [top16, tau], [tm8])
            tt("dve", e1[:, :, :], s0v, tm8[:, :].unsqueeze(2).to_broadcast([128, 8, 128]), ALU.add,
               [s_all, tm8], [e1])
            act(rf[:, 1, :, :], e1[:, :, :], AF.Exp, [e1], [rf])
            tt("dve", kp[:, :], tau[:, :], c24[:, :, 0], ALU.subtract, [tau, c24], [kp])
            act(kp[:, :], kp[:, :], AF.Exp, [kp], [kp])
            tt("dve", kp[:, :], kp[:, :], Zsum[:, :], ALU.mult, [kp, Zsum], [kp])
            dma("pool", rt_f_d[n], rf[:, :, :, :], [rf], [("rtf", n)], rf)
            dma("pool", kap_d[n], kp[:, :], [kp], [("kap", n)], kp)
        S.barrier()

    if debug == "rt":
        top.close()
        S.emit()
        return nc

    with contextlib.ExitStack() as es:
        ur = Rot([T(es, "p6u%d" % i, [128, D]) for i in range(2)])
        vr = Rot([T(es, "p6v%d" % i, [128, D]) for i in range(2)])
        ubr = Rot([T(es, "p6ub%d" % i, [128, D], BF16) for i in range(2)])
        vbr = Rot([T(es, "p6vb%d" % i, [128, D], BF16) for i in range(2)])
        uTr = Rot([T(es, "p6uT%d" % i, [128, 8, 128], BF16) for i in range(2)])
        pTr = Rot([P(es, "p6pT%d" % i, [128, 8, 128], BF16) for i in range(2)])
        for i in range(NEXP_BLK):
            u_t = ur.next(); v_t = vr.next(); ub = ubr.next(); vb = vbr.next(); uT = uTr.next()
            pT = pTr.next()
            dma("sp", u_t[:, :], peer_u[i * 128:(i + 1) * 128, :], [], [u_t], u_t)
            dma("sp", v_t[:, :], peer_v[i * 128:(i + 1) * 128, :], [], [v_t], v_t)
            act(ub[:, :], u_t[:, :], AF.Copy, [u_t], [ub])
            for kc in range(8):
                tr(pT[:, kc, :], ub[:, kc * 128:(kc + 1) * 128], identb[:, :], [ub, identb], [pT])
            cp("dve", uT[:, :, :], pT[:, :, :], [pT], [uT])
            dma("pool", uT_d[i], uT[:, :, :], [uT], [("uT", i)], uT)
            cp("dve", vb[:, :], v_t[:, :], [v_t], [vb])
            dma("pool", vB_d[i], vb[:, :], [vb], [("vB", i)], vb)
        S.barrier()

    if debug == "5a":
        top.close()
        S.emit()
        return nc

    IG = 4
    with contextlib.ExitStack() as es:
        h2t = T(es, "p7h2t", [128, 8, 512], BF16)
        rf = T(es, "p7rf", [128, 4, 2, 8, 128], F32)
        kp = T(es, "p7kp", [128, 4, 8], F32)
        dg = T(es, "p7dg", [128, 4, 8, 128], BF16)
        yacc = T(es, "p7yacc", [128, 4, D], F32)
        uTg_r = Rot([T(es, "p7uT%d" % i, [128, IG, 1024], BF16) for i in range(2)])
        vBg_r = Rot([T(es, "p7vB%d" % i, [128, IG, 1024], BF16) for i in range(2)])
        ga2_r = Rot([T(es, "p7ga2%d" % i, [128, IG, 512], BF16) for i in range(2)])
        eD_r = Rot([T(es, "p7eD%d" % i, [128, 4, 8, 128], F32) for i in range(2)])
        r1_r = Rot([T(es, "p7r1%d" % i, [128, 4, 8, 128], BF16) for i in range(2)])
        ga_r = Rot([T(es, "p7ga%d" % i, [128, 512]) for i in range(2)])
        x1t = T(es, "p7x1", [128, D]); x2 = T(es, "p7x2", [128, D])
        junk = T(es, "p7junk", [128, D], BF16)
        ss = T(es, "p7ss", [128, 1]); rstd = T(es, "p7rstd", [128, 1])
        yo = Rot([T(es, "p7yo%d" % i, [128, D]) for i in range(2)])
        Pa = Rot([P(es, "p7pa%d" % i, [128, 512]) for i in range(2)])
        Pg = Rot([P(es, "p7pg%d" % i, [128, 512]) for i in range(2)])
        Py = Rot([P(es, "p7py%d" % i, [128, 1024]) for i in range(2)])

        for tk in range(NTILE_PEER):
            for b in range(4):
                n = 4 * tk + b
                dma("sp", h2t[:, :, b * 128:(b + 1) * 128], h2T_d[n], [("h2T", n)], [h2t], h2t)
                dma("sp", rf[:, b, :, :, :], rt_f_d[n], [("rtf", n)], [rf], rf)
                dma("sp", kp[:, b, :], kap_d[n], [("kap", n)], [kp], kp)
            for b in range(4):
                for h in range(8):
                    ts("dve", dg[:, b, h, :], identb[:, :], kp[:, b, h:h + 1], None, ALU.mult, None,
                       [identb, kp], [dg])
            NI = NEXP_BLK
            grp = {}

            def get_group(ig):
                if ig not in grp:
                    uTg = uTg_r.next(); vBg = vBg_r.next(); ga2 = ga2_r.next()
                    i0 = ig * IG
                    dma("sp", uTg[:, :, :], uT_d[i0:i0 + IG].rearrange("i d k e -> d i (k e)"),
                        [("uT", i) for i in range(i0, i0 + IG)], [uTg], uTg)
                    dma("sp", vBg[:, :, :], vB_d[i0:i0 + IG].rearrange("i e d -> e i d"),
                        [("vB", i) for i in range(i0, i0 + IG)], [vBg], vBg)
                    grp[ig] = (uTg, vBg, ga2)
                return grp[ig]

            stU = {}

            def emit_U(i):
                uTg, vBg, ga2 = get_group(i // IG)
                ii = i % IG
                pa = Pa.next(); ga = ga_r.next()
                for kc in range(8):
                    mm(pa[:, :], uTg[:, ii, kc * 128:(kc + 1) * 128], h2t[:, kc, :],
                       kc == 0, kc == 7, [uTg, h2t], [pa])
                act(ga[:, :], pa[:, :], AF.Gelu_apprx_tanh, [pa], [ga])
                stU[i] = ga

            def emit_gate(i):
                eD = eD_r.next(); r1 = r1_r.next()
                slot = id(eD)
                keys = {b: [] for b in range(4)}
                kk = ("eD", slot, "p3"); keys[3].append(kk)
                tt("pool", eD[:, 3, :, :], rf[:, 3, 0, :, :],
                   rf[:, 3, 1, :, i:i + 1].to_broadcast([128, 8, 128]), ALU.mult, [rf], [kk])
                kk = ("eD", slot, "p2"); keys[2].append(kk)
                tt("pool", eD[:, 2, 4:8, :], rf[:, 2, 0, 4:8, :],
                   rf[:, 2, 1, 4:8, i:i + 1].to_broadcast([128, 4, 128]), ALU.mult, [rf], [kk])
                for b in range(3):
                    for h in range(8):
                        if b == 2 and h >= 4:
                            continue
                        kk = ("eD", slot, b, h)
                        keys[b].append(kk)
                        act(eD[:, b, h, :], rf[:, b, 0, h, :], AF.Copy, [rf], [kk],
                            scale=rf[:, b, 1, h, i:i + 1])
                for b in (3, 0, 1, 2):
                    stt("dve", r1[:, b, :, :], eD[:, b, :, :], 1.0, eD[:, b, :, :], ALU.is_ge,
                        ALU.mult, keys[b], [("r1", id(r1), b)])
                return r1

            def emit_diag(i, r1):
                uTg, vBg, ga2 = get_group(i // IG)
                ii = i % IG
                pgt = Pg.next()
                for b in (3, 0, 1, 2):
                    for h in range(8):
                        mm(pgt[:, b * 128:(b + 1) * 128], r1[:, b, h, :], dg[:, b, h, :],
                           h == 0, h == 7, [("r1", id(r1), b), dg], [pgt])
                ga = stU.pop(i)
                tt("dve", ga2[:, ii, :], pgt[:, :], ga[:, :], ALU.mult, [pgt, ga], [ga2])

            def emit_V(ig):
                uTg, vBg, ga2 = grp.pop(ig)
                for b in range(4):
                    py = Py.next()
                    for ii in range(IG):
                        for nh in range(2):
                            mm(py[:, nh * 512:(nh + 1) * 512], ga2[:, ii, b * 128:(b + 1) * 128],
                               vBg[:, ii, nh * 512:(nh + 1) * 512], ii == 0, ii == IG - 1,
                               [ga2, vBg], [py])
                    if ig == 0:
                        cp("dve", yacc[:, b, :], py[:, :], [py], [yacc])
                    else:
                        tt("dve", yacc[:, b, :], py[:, :], yacc[:, b, :], ALU.add, [py, yacc], [yacc])

            emit_U(0)
            for i in range(NI):
                r1 = emit_gate(i)
                if i + 1 < NI:
                    emit_U(i + 1)
                emit_diag(i, r1)
                if i % IG == IG - 1:
                    emit_V(i // IG)
            for b in range(4):
                n = 4 * tk + b
                y_t = yo.next()
                dma("sp", x1t[:, :], x1_d[n], [("x1", n)], [x1t], x1t)
                tt("dve", x2[:, :], yacc[:, b, :], GT2b[:, :], ALU.mult, [yacc, GT2b], [x2])
                tt("dve", x2[:, :], x2[:, :], x1t[:, :], ALU.add, [x2, x1t], [x2])
                act(junk[:, :], x2[:, :], AF.Square, [x2], [junk, ss], accum=ss[:, :])
                act(rstd[:, :], ss[:, :], AF.Sqrt, [ss, epsT], [rstd], bias=epsT[:, :], scale=1.0 / D)
                S.op("dve", lambda e: e.reciprocal(out=rstd[:, :], in_=rstd[:, :]), [rstd], [rstd])
                stt("dve", y_t[:, :], x2[:, :], rstd[:, 0:1], GFb[:, :], ALU.mult, ALU.mult,
                    [x2, rstd, GFb], [y_t])
                dma("pool", y_out[n * 128:(n + 1) * 128, :], y_t[:, :], [y_t], [], y_t)

    top.close()
    build_nc.stats = {e: len(S.q[e]) for e in ENGS}
    S.emit()
    return nc


def _consts():
    a = np.arange(128)
    ident = np.eye(128, dtype=np.float32)
    tri_incl = (a[:, None] >= a[None, :]).astype(np.float32)
    tri_low = (a[:, None] < a[None, :]).astype(np.float32)
    diagmask = (a[:, None] < a[None, :]).astype(np.float32)
    masks = []
    for r, nkb in DIL:
        for dl in range(nkb):
            d = 128 * dl + a[None, :] - a[:, None]
            masks.append(((d >= 0) & (d % r == 0) & (d <= 128 * r)).astype(np.float32))
    dilmask = np.stack(masks, axis=1)
    return ident, tri_incl, tri_low, diagmask, np.ascontiguousarray(dilmask)


def _rope_tables(shift):
    half = 32
    inv = (10000.0 ** (-np.arange(half, dtype=np.float32) / half)).astype(np.float32)
    pos = (np.arange(SEQ) - shift).astype(np.float32)
    ang = pos[:, None] * inv[None, :]
    cos = np.cos(ang).astype(np.float32).T
    sin = np.sin(ang).astype(np.float32).T
    C64 = np.concatenate([cos, cos], axis=0)
    S64 = np.concatenate([-sin, sin], axis=0)
    return (np.ascontiguousarray(np.concatenate([C64, C64], axis=0)),
            np.ascontiguousarray(np.concatenate([S64, S64], axis=0)))


def make_in_maps(x, c, w_ada, b_ada, g_mix, w_in, w_sb_o, w_dil_o, w_out, g_ffn, w_pq,
                 peer_keys, peer_u, peer_v, g_final):
    f = lambda a: np.ascontiguousarray(np.asarray(a, dtype=np.float32))
    x = f(x); c = f(c)
    w_in0 = f(w_in[0])
    perm = np.arange(1536)
    hd = perm % 64
    perm = perm - hd + (hd + 32) % 64
    w_perm = np.ascontiguousarray(w_in0[:, 1536:3072][:, perm])
    keysT = np.ascontiguousarray(
        np.transpose(f(peer_keys[0]).reshape(16, 128, 128), (2, 0, 1)))
    ident, tri_incl, tri_low, diagmask, dilmask = _consts()
    shared = {
        "w_ada": f(w_ada[0]), "b_ada": f(b_ada[0]).reshape(1, -1),
        "g_mix": f(g_mix[0]).reshape(1, -1), "g_ffn": f(g_ffn[0]).reshape(1, -1),
        "g_final": f(g_final).reshape(1, -1),
        "w_in": w_in0, "w_perm": w_perm, "w_sb_o": f(w_sb_o[0]), "w_dil_o": f(w_dil_o[0]),
        "w_out": f(w_out[0]), "w_pq": f(w_pq[0]), "keysT": keysT,
        "peer_u": f(peer_u[0]), "peer_v": f(peer_v[0]),
        "ident": ident, "tri_incl": tri_incl, "tri_low": tri_low, "diagmask": diagmask,
        "dilmask": dilmask,
    }
    ropes = {hh: _rope_tables(128 * (1 - hh)) for hh in (0, 1)}
    in_maps = []
    for core in range(8):
        b, hh = core // 2, core % 2
        if hh == 1:
            xall = x[b]
        else:
            xall = np.concatenate([np.zeros((128, D), np.float32), x[b, :SEQ - 128]], axis=0)
        m = dict(shared)
        m["xall"] = np.ascontiguousarray(xall)
        m["cT"] = np.ascontiguousarray(c[b].reshape(8, 128).T)
        m["kvalid"] = np.full((128, 1), float(hh), np.float32)
        m["ropeC"], m["ropeS"] = ropes[hh]
        in_maps.append(m)
    return in_maps


def kernel(**inputs):
    in_maps = make_in_maps(**inputs)
    nc = build_nc()
    res = run_bass_kernel_spmd(nc, in_maps, core_ids=list(range(8)))
    out = np.zeros((4, SEQ, D), np.float32)
    for core in range(8):
        b, hh = core // 2, core % 2
        y = np.asarray(res.results[core]["y"]).reshape(NOWN, 128, D)
        out[b].reshape(NB, 128, D)[hh::2] = y
    return out
```
